# Optimizing a Trainium2 kernel written in Bass

```python
import math
import jax
import jax.numpy as jnp
from jax import lax
import numpy as np

D_MODEL = 2048
BATCH = 4
SEQ = 2048
DEPTH = 2

GRID_W = 64
CTX_LEN = 256
EPS = 1e-6
NEG_INF = -1e30
ROPE_BASE = 10000.0
Q_BLOCK = 128

DA_HEADS = 4
DA_QK_DIM = 64
DA_V_DIM = 2 * DA_QK_DIM
WB_HEADS = 8
WB_KV_HEADS = 2
WB_DIM = 64
WB_WINDOW = 128
WB_BLOCK = 128
NA_HEADS = 8
NA_DIM = 64
NA_ROWS = 8
NA_COLS = 16
S5_GROUPS = 32
S5_GROUP_CH = 16
S5_STATE = 64
S5_WIDTH = S5_GROUPS * S5_GROUP_CH
N_BRANCH = 4
BRANCH_WIDTH = 512
N_EXPERTS = 16
EXPERT_FF = 2048
EC_CAPACITY_FACTOR = 2

IN_WIDTHS = (
    2 * DA_HEADS * DA_QK_DIM, 2 * DA_HEADS * DA_QK_DIM, DA_HEADS * DA_V_DIM,
    WB_HEADS * WB_DIM, WB_KV_HEADS * WB_DIM, WB_KV_HEADS * WB_DIM,
    NA_HEADS * NA_DIM, NA_HEADS * NA_DIM, NA_HEADS * NA_DIM,
    S5_WIDTH,
)
N_MIX_IN = sum(IN_WIDTHS)
N_IN = N_MIX_IN + N_BRANCH * D_MODEL

kernel_name = 'hybrid_flow_block'


def rmsnorm(x, g):
    xf = x.astype(jnp.float32)
    y = xf * lax.rsqrt(jnp.mean(xf * xf, axis=-1, keepdims=True) + EPS)
    return (y * g.astype(jnp.float32)).astype(x.dtype)


def modulate(h, shift, scale):
    return h * (1.0 + scale) + shift


def split_cols(z, widths):
    return jnp.split(z, np.cumsum(widths)[:-1].tolist(), axis=-1)


def _rotate(x, pos):
    d = x.shape[-1]
    inv_freq = jnp.power(ROPE_BASE, -jnp.arange(0, d, 2, dtype=jnp.float32) / d)
    ang = pos.astype(jnp.float32)[:, None] * inv_freq[None, :]
    ang = ang.reshape((ang.shape[0],) + (1,) * (x.ndim - 3) + (d // 2,))
    cos, sin = jnp.cos(ang).astype(x.dtype), jnp.sin(ang).astype(x.dtype)
    x1, x2 = x[..., : d // 2], x[..., d // 2:]
    return jnp.concatenate([x1 * cos - x2 * sin, x2 * cos + x1 * sin], axis=-1)


def axial_rope(x, rows, cols):
    half = x.shape[-1] // 2
    return jnp.concatenate([_rotate(x[..., :half], rows), _rotate(x[..., half:], cols)], axis=-1)


def diff_attention(q_x, k_x, v_x, q_c, k_c, v_c, lam_vec, subln_g, layer, rows, cols, with_ctx):
    b, n, _ = q_x.shape
    heads = lambda t: t.reshape(t.shape[0], t.shape[1], DA_HEADS, 2, DA_QK_DIM)
    vheads = lambda t: t.reshape(t.shape[0], t.shape[1], DA_HEADS, DA_V_DIM)
    lam_init = 0.8 - 0.6 * math.exp(-0.3 * layer)
    lam = (jnp.exp(jnp.sum(lam_vec[0] * lam_vec[1])) - jnp.exp(jnp.sum(lam_vec[2] * lam_vec[3]))).astype(jnp.float32) + lam_init
    scale = DA_QK_DIM ** -0.5
    k_ctx, v_ctx = heads(k_c), vheads(v_c)
    k_all = jnp.concatenate([k_ctx, axial_rope(heads(k_x), rows, cols)], axis=1)
    v_all = jnp.concatenate([v_ctx, vheads(v_x)], axis=1)

    def attend(q, k, v):
        s = jnp.einsum('bqhmd,bkhmd->bhmqk', q, k).astype(jnp.float32) * scale
        p = jax.nn.softmax(s, axis=-1)
        w = p[:, :, 0] - lam * p[:, :, 1]
        return jnp.einsum('bhqk,bkhd->bqhd', w.astype(v.dtype), v)

    def post(o):
        return (rmsnorm(o, subln_g) * (1.0 - lam_init)).reshape(o.shape[0], o.shape[1], DA_HEADS * DA_V_DIM)

    nb = n // Q_BLOCK
    q_lat = axial_rope(heads(q_x), rows, cols).reshape(b, nb, Q_BLOCK, DA_HEADS, 2, DA_QK_DIM)
    o = lax.map(lambda qb: attend(qb, k_all, v_all), jnp.moveaxis(q_lat, 1, 0))
    y_x = post(jnp.moveaxis(o, 0, 1).reshape(b, n, DA_HEADS, DA_V_DIM))
    y_c = post(attend(heads(q_c), k_ctx, v_ctx)) if with_ctx else None
    return y_x, y_c


def window_gqa(q_x, k_x, v_x, q_c, k_c, v_c, sink, rows, cols, with_ctx):
    b, n, _ = q_x.shape
    g, r = WB_KV_HEADS, WB_HEADS // WB_KV_HEADS
    nb, blk, d = n // WB_BLOCK, WB_BLOCK, WB_DIM
    scale = d ** -0.5
    sink_gr = sink.astype(jnp.float32).reshape(g, r)
    q_lat = axial_rope(q_x.reshape(b, n, g, r, d), rows, cols).reshape(b, nb, blk, g, r, d)
    k_lat = axial_rope(k_x.reshape(b, n, g, d), rows, cols)
    v_lat = v_x.reshape(b, n, g, d)
    k_ctx = k_c.reshape(b, -1, g, d)
    v_ctx = v_c.reshape(b, -1, g, d)
    n_ctx = k_ctx.shape[1]

    def band(t):
        tp = jnp.pad(t.reshape(b, nb, blk, g, d), ((0, 0), (1, 1), (0, 0), (0, 0), (0, 0)))
        return jnp.concatenate([tp[:, :-2], tp[:, 1:-1], tp[:, 2:]], axis=2)

    k_band, v_band = band(k_lat), band(v_lat)
    blk_id = jnp.arange(nb)[:, None, None]
    q_pos = blk_id * blk + jnp.arange(blk)[None, :, None]
    k_pos = (blk_id - 1) * blk + jnp.arange(3 * blk)[None, None, :]
    valid = (jnp.abs(q_pos - k_pos) <= WB_WINDOW) & (k_pos >= 0) & (k_pos < n)
    s_band = jnp.einsum('bnqgrd,bnkgd->bngrqk', q_lat, k_band).astype(jnp.float32) * scale
    s_band = jnp.where(valid[None, :, None, None], s_band, NEG_INF)
    s_ctx = jnp.einsum('bnqgrd,bcgd->bngrqc', q_lat, k_ctx).astype(jnp.float32) * scale
    s_sink = jnp.broadcast_to(sink_gr[None, None, :, :, None, None], s_ctx.shape[:-1] + (1,))
    p = jax.nn.softmax(jnp.concatenate([s_band, s_ctx, s_sink], axis=-1), axis=-1)
    p_band = p[..., :3 * blk].astype(v_lat.dtype)
    p_ctx = p[..., 3 * blk:3 * blk + n_ctx].astype(v_lat.dtype)
    o = jnp.einsum('bngrqk,bnkgd->bnqgrd', p_band, v_band) + jnp.einsum('bngrqc,bcgd->bnqgrd', p_ctx, v_ctx)
    y_x = o.reshape(b, n, WB_HEADS * d)
    y_c = None
    if with_ctx:
        q_ctx = q_c.reshape(b, n_ctx, g, r, d)
        s = jnp.einsum('bqgrd,bcgd->bgrqc', q_ctx, k_ctx).astype(jnp.float32) * scale
        s_sink_c = jnp.broadcast_to(sink_gr[None, :, :, None, None], s.shape[:-1] + (1,))
        pc = jax.nn.softmax(jnp.concatenate([s, s_sink_c], axis=-1), axis=-1)[..., :n_ctx]
        y_c = jnp.einsum('bgrqc,bcgd->bqgrd', pc.astype(v_ctx.dtype), v_ctx).reshape(b, n_ctx, WB_HEADS * d)
    return y_x, y_c


def neighbourhood_attention(q_x, k_x, v_x, q_c, k_c, v_c, rpb, with_ctx):
    b, n, _ = q_x.shape
    n_rows = n // GRID_W
    kh = min(NA_ROWS, n_rows)
    h, d = NA_HEADS, NA_DIM
    scale = d ** -0.5
    grid = lambda t: t.reshape(b, n_rows, GRID_W, h, d)
    q_g, k_g, v_g = grid(q_x), grid(k_x), grid(v_x)
    k_ctx = k_c.reshape(b, -1, h, d)
    v_ctx = v_c.reshape(b, -1, h, d)
    n_ctx = k_ctx.shape[1]
    r = jnp.arange(n_rows)
    row_idx = jnp.clip(r - kh // 2, 0, n_rows - kh)[:, None] + jnp.arange(kh)[None, :]
    k_nb, v_nb = k_g[:, row_idx], v_g[:, row_idx]
    col = jnp.arange(GRID_W)
    col_start = jnp.clip(col - NA_COLS // 2, 0, GRID_W - NA_COLS)
    col_valid = (col[None, :] >= col_start[:, None]) & (col[None, :] < col_start[:, None] + NA_COLS)
    dr = row_idx - r[:, None] + (NA_ROWS - 1)
    dc = jnp.clip(col[None, :] - col[:, None], 1 - NA_COLS, NA_COLS - 1) + (NA_COLS - 1)
    bias = rpb.astype(jnp.float32)[:, dr[:, :, None, None], dc[None, None, :, :]]
    bias = jnp.transpose(bias, (1, 0, 3, 2, 4))
    s = jnp.einsum('brchd,brkwhd->brhckw', q_g, k_nb).astype(jnp.float32) * scale + bias[None]
    s = jnp.where(col_valid[:, None, :], s, NEG_INF).reshape(b, n_rows, h, GRID_W, kh * GRID_W)
    s_ctx = jnp.einsum('brchd,bshd->brhcs', q_g, k_ctx).astype(jnp.float32) * scale
    p = jax.nn.softmax(jnp.concatenate([s, s_ctx], axis=-1), axis=-1).astype(v_g.dtype)
    p_nb = p[..., :kh * GRID_W].reshape(b, n_rows, h, GRID_W, kh, GRID_W)
    o = jnp.einsum('brhckw,brkwhd->brchd', p_nb, v_nb) + jnp.einsum('brhcs,bshd->brchd', p[..., kh * GRID_W:], v_ctx)
    y_x = o.reshape(b, n, h * d)
    y_c = None
    if with_ctx:
        sc = jnp.einsum('bqhd,bkhd->bhqk', q_c.reshape(b, n_ctx, h, d), k_ctx).astype(jnp.float32) * scale
        pc = jax.nn.softmax(sc, axis=-1).astype(v_ctx.dtype)
        y_c = jnp.einsum('bhqk,bkhd->bqhd', pc, v_ctx).reshape(b, n_ctx, h * d)
    return y_x, y_c


def _diag_scan(a, bu, reverse):
    def combine(e1, e2):
        a1, b1 = e1
        a2, b2 = e2
        return a1 * a2, a2 * b1 + b2
    return lax.associative_scan(combine, (a, bu), reverse=reverse, axis=0)[1]


def s5_bidirectional(u_x, u_c, a_re, a_im, log_step, b_re, b_im, c_re, c_im, d_skip, w_glu, with_ctx):
    f32 = jnp.float32
    n_lat, n_ctx = u_x.shape[1], u_c.shape[1]

    def time_major(u):
        return jnp.moveaxis(u.astype(f32).reshape(u.shape[0], u.shape[1], S5_GROUPS, S5_GROUP_CH), 1, 0).astype(jnp.complex64)

    ux, uc = time_major(u_x), time_major(u_c)
    ys_x, ys_c = [], []
    for direction in range(2):
        reverse = direction == 1
        lam = lax.complex(a_re[direction].astype(f32), a_im[direction].astype(f32))
        lam_dt = lam * jnp.exp(log_step[direction].astype(f32))[:, None]
        a_bar = jnp.exp(lam_dt)
        b_bar = ((a_bar - 1.0) / lam)[:, :, None] * lax.complex(b_re[direction].astype(f32), b_im[direction].astype(f32))
        c_mat = lax.complex(c_re[direction].astype(f32), c_im[direction].astype(f32))
        h_c = _diag_scan(jnp.broadcast_to(a_bar, (n_ctx, 1) + a_bar.shape), jnp.einsum('tbgh,gph->tbgp', uc, b_bar), reverse)
        h0 = h_c[0] if reverse else h_c[-1]
        steps = jnp.arange(n_lat, 0, -1, dtype=f32) if reverse else jnp.arange(1, n_lat + 1, dtype=f32)
        carry = jnp.exp(lam_dt * steps[:, None, None])[:, None] * h0[None]
        h_x = _diag_scan(jnp.broadcast_to(a_bar, (n_lat, 1) + a_bar.shape), jnp.einsum('tbgh,gph->tbgp', ux, b_bar), reverse) + carry
        ys_x.append(jnp.einsum('tbgp,ghp->tbgh', h_x, c_mat).real)
        if with_ctx:
            ys_c.append(jnp.einsum('tbgp,ghp->tbgh', h_c, c_mat).real)

    def out(y, u):
        y = jnp.moveaxis(y, 0, 1).reshape(u.shape[0], u.shape[1], S5_WIDTH) + d_skip.astype(f32) * u.astype(f32)
        g = jax.nn.gelu(y)
        return (g * jax.nn.sigmoid(g @ w_glu.astype(f32))).astype(u.dtype)

    y_x = out(ys_x[0] + ys_x[1], u_x)
    y_c = out(ys_c[0] + ys_c[1], u_c) if with_ctx else None
    return y_x, y_c


def merge_branches(branches, gate_logits, w_branch, w_out):
    y = jnp.stack(branches, axis=2)
    proj = jnp.einsum('btnk,nkd->btnd', y, w_branch)
    gates = jax.nn.sigmoid(gate_logits.reshape(gate_logits.shape[0], gate_logits.shape[1], N_BRANCH, D_MODEL))
    return jnp.sum(gates * proj, axis=2) @ w_out


def expert_choice_moe(h, w_router, w1, w3, w2):
    b, t, _ = h.shape
    cap = EC_CAPACITY_FACTOR * t // N_EXPERTS
    aff = jax.nn.softmax(jnp.einsum('btd,de->bte', h, w_router).astype(jnp.float32), axis=-1)
    gate, idx = lax.top_k(jnp.swapaxes(aff, 1, 2), cap)
    bidx = jnp.arange(b)[:, None, None]
    xs = h[bidx, idx]
    a = jnp.einsum('becd,edf->becf', xs, w1)
    g = jnp.einsum('becd,edf->becf', xs, w3)
    out = jnp.einsum('becf,efd->becd', jax.nn.silu(a) * g, w2)
    out = out * gate[..., None].astype(out.dtype)
    return jnp.zeros_like(h).at[bidx, idx].add(out.astype(h.dtype))


def setup_inputs(seed: int = 0) -> dict:
    key = jax.random.key(seed)
    ks = jax.random.split(key, 30)
    f32 = jnp.float32

    def nrm(i, shape, scale):
        return jax.random.normal(ks[i], shape, f32) * scale

    s5_ap = (DEPTH, 2, S5_GROUPS, S5_STATE)
    return {
        'x': nrm(0, (BATCH, SEQ, D_MODEL), 1.0),
        'c': nrm(1, (BATCH, D_MODEL), 1.0),
        'ctx': nrm(2, (BATCH, CTX_LEN, D_MODEL), 1.0),
        'c_ctx': nrm(3, (D_MODEL,), 1.0),
        'ada_w': nrm(4, (DEPTH, D_MODEL, 6 * D_MODEL), 0.5 * D_MODEL ** -0.5),
        'ada_b': nrm(5, (DEPTH, 6 * D_MODEL), 0.02),
        'norm1_g': 1.0 + nrm(6, (DEPTH, D_MODEL), 0.02),
        'norm2_g': 1.0 + nrm(7, (DEPTH, D_MODEL), 0.02),
        'w_in': nrm(8, (DEPTH, D_MODEL, N_IN), D_MODEL ** -0.5),
        'da_lambda': nrm(9, (DEPTH, 4, DA_QK_DIM), 0.1),
        'da_subln_g': 1.0 + nrm(10, (DEPTH, DA_V_DIM), 0.02),
        'wb_sink': nrm(11, (DEPTH, WB_HEADS), 0.5),
        'na_rpb': nrm(12, (DEPTH, NA_HEADS, 2 * NA_ROWS - 1, 2 * NA_COLS - 1), 0.1),
        's5_a_re': -0.5 + nrm(13, s5_ap, 0.01),
        's5_a_im': jnp.pi * jnp.arange(S5_STATE, dtype=f32) + nrm(14, s5_ap, 0.01),
        's5_log_step': jax.random.uniform(ks[15], (DEPTH, 2, S5_GROUPS), f32, math.log(1e-3), math.log(1e-1)),
        's5_b_re': nrm(16, (DEPTH, 2, S5_GROUPS, S5_STATE, S5_GROUP_CH), (2 * S5_GROUP_CH) ** -0.5),
        's5_b_im': nrm(17, (DEPTH, 2, S5_GROUPS, S5_STATE, S5_GROUP_CH), (2 * S5_GROUP_CH) ** -0.5),
        's5_c_re': nrm(18, (DEPTH, 2, S5_GROUPS, S5_GROUP_CH, S5_STATE), S5_STATE ** -0.5),
        's5_c_im': nrm(19, (DEPTH, 2, S5_GROUPS, S5_GROUP_CH, S5_STATE), S5_STATE ** -0.5),
        's5_d': nrm(20, (DEPTH, S5_WIDTH), 1.0),
        's5_glu_w': nrm(21, (DEPTH, S5_WIDTH, S5_WIDTH), S5_WIDTH ** -0.5),
        'w_branch': nrm(22, (DEPTH, N_BRANCH, BRANCH_WIDTH, D_MODEL), BRANCH_WIDTH ** -0.5),
        'w_out': nrm(23, (DEPTH, D_MODEL, D_MODEL), D_MODEL ** -0.5),
        'w_router': nrm(24, (DEPTH, D_MODEL, N_EXPERTS), D_MODEL ** -0.5),
        'w_e1': nrm(25, (DEPTH, N_EXPERTS, D_MODEL, EXPERT_FF), D_MODEL ** -0.5),
        'w_e3': nrm(26, (DEPTH, N_EXPERTS, D_MODEL, EXPERT_FF), D_MODEL ** -0.5),
        'w_e2': nrm(27, (DEPTH, N_EXPERTS, EXPERT_FF, D_MODEL), EXPERT_FF ** -0.5),
        'final_g': 1.0 + nrm(28, (D_MODEL,), 0.02),
    }


def reference(x, c, ctx, c_ctx, ada_w, ada_b, norm1_g, norm2_g, w_in, da_lambda, da_subln_g, wb_sink, na_rpb,
              s5_a_re, s5_a_im, s5_log_step, s5_b_re, s5_b_im, s5_c_re, s5_c_im, s5_d, s5_glu_w,
              w_branch, w_out, w_router, w_e1, w_e3, w_e2, final_g):
    n_lat = x.shape[1]
    pos = jnp.arange(n_lat)
    rows, cols = pos // GRID_W, pos % GRID_W
    silu_c, silu_cc = jax.nn.silu(c), jax.nn.silu(c_ctx)
    for l in range(DEPTH):
        with_ctx = l < DEPTH - 1
        mod_x = jnp.split((silu_c @ ada_w[l] + ada_b[l])[:, None, :], 6, axis=-1)
        mod_c = jnp.split(silu_cc @ ada_w[l] + ada_b[l], 6, axis=-1)
        hx = modulate(rmsnorm(x, norm1_g[l]), mod_x[0], mod_x[1])
        hc = modulate(rmsnorm(ctx, norm1_g[l]), mod_c[0], mod_c[1])
        w_in_l = w_in[l]
        zx = hx @ w_in_l
        zc = hc @ (w_in_l if with_ctx else w_in_l[:, :N_MIX_IN])
        px = split_cols(zx[..., :N_MIX_IN], IN_WIDTHS)
        pc = split_cols(zc[..., :N_MIX_IN], IN_WIDTHS)
        ya = diff_attention(px[0], px[1], px[2], pc[0], pc[1], pc[2], da_lambda[l], da_subln_g[l], l, rows, cols, with_ctx)
        yb = window_gqa(px[3], px[4], px[5], pc[3], pc[4], pc[5], wb_sink[l], rows, cols, with_ctx)
        yc = neighbourhood_attention(px[6], px[7], px[8], pc[6], pc[7], pc[8], na_rpb[l], with_ctx)
        yd = s5_bidirectional(px[9], pc[9], s5_a_re[l], s5_a_im[l], s5_log_step[l], s5_b_re[l], s5_b_im[l],
                              s5_c_re[l], s5_c_im[l], s5_d[l], s5_glu_w[l], with_ctx)
        x = x + mod_x[2] * merge_branches([ya[0], yb[0], yc[0], yd[0]], zx[..., N_MIX_IN:], w_branch[l], w_out[l])
        if with_ctx:
            ctx = ctx + mod_c[2] * merge_branches([ya[1], yb[1], yc[1], yd[1]], zc[..., N_MIX_IN:], w_branch[l], w_out[l])
        hx = modulate(rmsnorm(x, norm2_g[l]), mod_x[3], mod_x[4])
        x = x + mod_x[5] * expert_choice_moe(hx, w_router[l], w_e1[l], w_e3[l], w_e2[l])
        if with_ctx:
            hc = modulate(rmsnorm(ctx, norm2_g[l]), mod_c[3], mod_c[4])
            ctx = ctx + mod_c[5] * expert_choice_moe(hc, w_router[l], w_e1[l], w_e3[l], w_e2[l])
    return rmsnorm(x, final_g)
```

```python
import math
from contextlib import ExitStack

import numpy as np
import ml_dtypes

import concourse.bass as bass
import concourse.mybir as mybir
from concourse.bass_utils import run_bass_kernel_spmd

F32 = mybir.dt.float32
BF16 = mybir.dt.bfloat16
AF = mybir.ActivationFunctionType
ALU = mybir.AluOpType
AX = mybir.AxisListType
NPBF = ml_dtypes.bfloat16

D = 2048
KC = D // 128
EPS = 1e-6
EPOCH = 2048
NSLOT = 12


class Buf:
    __slots__ = ("t", "w", "r", "name")

    def __init__(self, t, name):
        self.t = t
        self.w = None
        self.r = []
        self.name = name

    def __getitem__(self, idx):
        return self.t[idx]


class Prog:
    ENG = ("pe", "act", "dve", "pool", "sp")

    def __init__(self):
        self.nc = bass.Bass("TRN2", target_bir_lowering=False)
        self.es = ExitStack()
        self.ops = {e: [] for e in self.ENG}
        self.n = {e: 0 for e in self.ENG}
        self.sems = {}
        self.seen = {e: {} for e in self.ENG}
        self.slot_uses = {}
        self.ndma = {e: 0 for e in self.ENG}
        self.uid = 0

    def sem(self, key):
        if key not in self.sems:
            self.sems[key] = self.es.enter_context(self.nc.semaphore("s_" + "_".join(str(k) for k in key)))
        return self.sems[key]

    def sb(self, shape, dt, name=None):
        self.uid += 1
        name = (name or "sb") + "_%d" % self.uid
        return Buf(self.es.enter_context(self.nc.sbuf_tensor(name, list(shape), dt)), name)

    def ps(self, shape, dt=F32, name=None):
        self.uid += 1
        name = (name or "ps") + "_%d" % self.uid
        return Buf(self.es.enter_context(self.nc.psum_tensor(name, list(shape), dt)), name)

    def dram(self, name, shape, dt, kind):
        return Buf(self.nc.dram_tensor(name, list(shape), dt, kind=kind).ap(), name)

    def _deps(self, eng, reads, writes):
        deps = []
        for b in reads:
            if b.w is not None:
                deps.append(b.w)
        for b in writes:
            if b.w is not None:
                deps.append(b.w)
            deps.extend(b.r)
        out = []
        for (deng, key, val) in deps:
            if eng == "pe" and deng == "pe":
                continue
            if self.seen[eng].get(key, 0) >= val:
                continue
            self.seen[eng][key] = val
            out.append((key, val))
        return out

    def _commit(self, ticket, reads, writes):
        for b in reads:
            if b not in writes:
                b.r.append(ticket)
        for b in writes:
            b.w = ticket
            b.r = []

    def op(self, eng, fn, reads=(), writes=()):
        reads = [b for b in reads if b is not None]
        writes = [b for b in writes if b is not None]
        waits = self._deps(eng, reads, writes)
        idx = self.n[eng]
        self.n[eng] += 1
        key = ("c", eng, idx // EPOCH)
        val = idx % EPOCH + 1
        self.sem(key)
        for k, _ in waits:
            self.sem(k)
        self.ops[eng].append((waits, fn, key, 1))
        self._commit((eng, key, val), reads, writes)

    def dma(self, q, out_ap, in_ap, reads=(), writes=()):
        reads = [b for b in reads if b is not None]
        writes = [b for b in writes if b is not None]
        waits = self._deps(q, reads, writes)
        slot = self.ndma[q] % NSLOT
        self.ndma[q] += 1
        key = ("d", q, slot)
        uses = self.slot_uses.get(key, 0)
        if uses > 0 and self.seen[q].get(key, 0) < 16 * uses:
            waits.append((key, 16 * uses))
            self.seen[q][key] = 16 * uses
        self.slot_uses[key] = uses + 1
        self.sem(key)
        for k, _ in waits:
            self.sem(k)
        self.ops[q].append((waits, lambda e: e.dma_start(out=out_ap, in_=in_ap), key, 16))
        self._commit((q + "_dma", key, 16 * (uses + 1)), reads, writes)

    def mm(self, out_b, out_ap, l_b, l_ap, r_b, r_ap, start=True, stop=True):
        self.op("pe", lambda e: e.matmul(out_ap, lhsT=l_ap, rhs=r_ap, start=start, stop=stop),
                reads=[l_b, r_b], writes=[out_b])

    def tr(self, out_b, out_ap, in_b, in_ap, id_b, id_ap):
        self.op("pe", lambda e: e.transpose(out_ap, in_ap, id_ap), reads=[in_b, id_b], writes=[out_b])

    def finish(self):
        for q in self.ENG:
            for (key, uses) in list(self.slot_uses.items()):
                if key[1] == q and self.seen[q].get(key, 0) < 16 * uses:
                    self.ops[q].append(([(key, 16 * uses)], None, None, 0))
        nc = self.nc
        with nc.Block() as block:
            def emit(eng_name):
                def body(e):
                    for (waits, fn, key, inc) in self.ops[eng_name]:
                        for (k, v) in waits:
                            e.wait_ge(self.sems[k], v)
                        if fn is not None:
                            fn(e).then_inc(self.sems[key], inc)
                return body
            block.tensor(emit("pe"))
            block.scalar(emit("act"))
            block.vector(emit("dve"))
            block.gpsimd(emit("pool"))
            block.sync(emit("sp"))
        self.es.close()
        return nc


def run(prog, in_maps, ncores=8):
    nc = prog.finish()
    res = run_bass_kernel_spmd(nc, in_maps, core_ids=list(range(ncores)))
    return res.results


def bcast_rows(ap2d, nparts):
    return ap2d.partition_broadcast(nparts)


ADA_SL = 12288 // 8


def build_ada():
    p = Prog()
    cT = p.dram("cT", [128, KC * 5], F32, "ExternalInput")
    wa = p.dram("wa", [2, D, ADA_SL], F32, "ExternalInput")
    ba = p.dram("ba", [2, 1, ADA_SL], F32, "ExternalInput")
    mod = p.dram("mod", [2, 5, ADA_SL], F32, "ExternalOutput")
    c_sb = p.sb([128, KC * 5], F32)
    s_sb = p.sb([128, KC * 5], F32)
    p.dma("sp", c_sb[:], cT[:, :], reads=[cT], writes=[c_sb])
    p.op("act", lambda e: e.activation(out=s_sb[:], in_=c_sb[:], func=AF.Silu), reads=[c_sb], writes=[s_sb])
    wbuf = [p.sb([128, KC, 512], F32, "wa") for _ in range(2)]
    bbuf = [p.sb([5, 512], F32, "ba") for _ in range(2)]
    obuf = [p.sb([5, 512], F32, "o") for _ in range(2)]
    pss = [p.ps([5, 512], F32) for _ in range(2)]
    it = 0
    for l in range(2):
        for j in range(ADA_SL // 512):
            w, b, o, ps = wbuf[it % 2], bbuf[it % 2], obuf[it % 2], pss[it % 2]
            it += 1
            cs = slice(j * 512, (j + 1) * 512)
            p.dma("sp", w[:], wa[l, :, cs].rearrange("(k p) c -> p k c", p=128), reads=[wa], writes=[w])
            p.dma("sp", b[:], ba[l, 0:1, cs].partition_broadcast(5), reads=[ba], writes=[b])
            for k in range(KC):
                p.mm(ps, ps[:], s_sb, s_sb[:, k * 5:(k + 1) * 5], w, w[:, k, :], start=(k == 0), stop=(k == KC - 1))
            p.op("dve", lambda e, o=o, ps=ps, b=b: e.tensor_tensor(out=o[:], in0=ps[:], in1=b[:], op=ALU.add),
                 reads=[ps, b], writes=[o])
            p.dma("sp", mod[l, :, cs], o[:], reads=[o], writes=[mod])
    return p


def run_ada(c, c_ctx, ada_w, ada_b):
    call = np.concatenate([c, c_ctx[None]], 0).astype(np.float32)
    cT = np.ascontiguousarray(call.T.reshape(KC, 128, 5).transpose(1, 0, 2).reshape(128, KC * 5))
    maps = []
    for i in range(8):
        sl = slice(i * ADA_SL, (i + 1) * ADA_SL)
        maps.append({"cT": cT, "wa": np.ascontiguousarray(ada_w[:, :, sl]),
                     "ba": np.ascontiguousarray(ada_b[:, None, sl])})
    res = run(build_ada(), maps)
    return np.concatenate([r["mod"] for r in res], axis=2)


class PsPool:
    def __init__(self, p, n, shape=(128, 512), dt=F32):
        self.t = [p.ps(list(shape), dt) for _ in range(n)]
        self.i = 0

    def next(self):
        t = self.t[self.i % len(self.t)]
        self.i += 1
        return t


def load_ident(p, ident_d):
    idf = p.sb([128, 128], F32, "idf")
    idb = p.sb([128, 128], BF16, "idb")
    p.dma("sp", idf[:], ident_d[:, :], reads=[ident_d], writes=[idf])
    p.op("dve", lambda e: e.tensor_copy(out=idb[:], in_=idf[:]), reads=[idf], writes=[idb])
    return idf, idb


class LN:
    def __init__(self, p):
        self.p = p
        self.junk = p.sb([128, D], F32, "lnj")
        self.ss = p.sb([128, 1], F32, "lnss")
        self.rstd = p.sb([128, 1], F32, "lnr")

    def run(self, x_b, x_ap, gmul, shift, out_b, out_ap):
        p, junk, ss, rstd = self.p, self.junk, self.ss, self.rstd
        p.op("act", lambda e: e.activation(out=junk[:], in_=x_ap, func=AF.Square), reads=[x_b], writes=[junk])
        p.op("dve", lambda e: e.reduce_sum(out=ss[:], in_=junk[:], axis=AX.X), reads=[junk], writes=[ss])
        p.op("act", lambda e: e.activation(out=rstd[:], in_=ss[:], func=AF.Sqrt, scale=1.0 / D, bias=EPS),
             reads=[ss], writes=[rstd])
        p.op("dve", lambda e: e.reciprocal(out=rstd[:], in_=rstd[:]), reads=[rstd], writes=[rstd])
        p.op("dve", lambda e: e.scalar_tensor_tensor(out=junk[:], in0=x_ap, scalar=rstd[:, 0:1], in1=gmul[:],
                                                     op0=ALU.mult, op1=ALU.mult),
             reads=[x_b, rstd, gmul], writes=[junk])
        p.op("pool", lambda e: e.tensor_tensor(out=out_ap, in0=junk[:], in1=shift[:], op=ALU.add),
             reads=[junk, shift], writes=[out_b])


def load_gmul_shift(p, g_d, mod_d, r_shift, r_scale, gmul, shift, tmp):
    p.dma("sp", tmp[:], mod_d[r_scale:r_scale + 1, :].partition_broadcast(128), reads=[mod_d], writes=[tmp])
    p.dma("sp", gmul[:], g_d[0:1, :].partition_broadcast(128), reads=[g_d], writes=[gmul])
    p.op("dve", lambda e: e.scalar_tensor_tensor(out=gmul[:], in0=tmp[:], scalar=1.0, in1=gmul[:],
                                                 op0=ALU.add, op1=ALU.mult), reads=[tmp, gmul], writes=[gmul])
    p.dma("sp", shift[:], mod_d[r_shift:r_shift + 1, :].partition_broadcast(128), reads=[mod_d], writes=[shift])


def transpose_to(p, src_b, src_tile_ap_fn, nblk, idb, pst_pool, dst_b, dst_ap_fn, eng="act"):
    for k0 in range(0, nblk, 4):
        n = min(4, nblk - k0)
        pst = pst_pool.next()
        for j in range(n):
            p.tr(pst, pst[:, j * 128:(j + 1) * 128], src_b, src_tile_ap_fn(k0 + j), idb, idb[:])
        dst = dst_ap_fn(k0, n)
        src = pst[:, 0:n * 128].rearrange("p (k t) -> p k t", t=128)
        if eng == "act":
            p.op("act", lambda e, dst=dst, src=src: e.copy(out=dst, in_=src), reads=[pst], writes=[dst_b])
        else:
            p.op("dve", lambda e, dst=dst, src=src: e.tensor_copy(out=dst, in_=src), reads=[pst], writes=[dst_b])


def build_merge(kinds, tbt=3):
    p = Prog()
    nt = len(kinds)
    NT = nt * 128
    xin = p.dram("xin", [NT, D], F32, "ExternalInput")
    yT = p.dram("yT", [D, NT], F32, "ExternalInput")
    modd = {"x": p.dram("modx", [6, D], F32, "ExternalInput"), "c": p.dram("modc", [6, D], F32, "ExternalInput")}
    g1 = p.dram("g1", [1, D], F32, "ExternalInput")
    g2 = p.dram("g2", [1, D], F32, "ExternalInput")
    wg = p.dram("wg", [16, D, 512], F32, "ExternalInput")
    wbr = p.dram("wbr", [16, D, 128], F32, "ExternalInput")
    wout = p.dram("wout", [D, D], F32, "ExternalInput")
    wglu = p.dram("wglu", [512, 512], F32, "ExternalInput")
    wr = p.dram("wr", [D, 16], F32, "ExternalInput")
    ident = p.dram("ident", [128, 128], F32, "ExternalInput")
    xnew = p.dram("xnew", [NT, D], F32, "ExternalOutput")
    hx2o = p.dram("hx2", [NT, D], BF16, "ExternalOutput")
    affo = p.dram("aff", [NT, 16], F32, "ExternalOutput")

    idf, idb = load_ident(p, ident)
    ln = LN(p)
    pp = PsPool(p, 5)
    ppt = PsPool(p, 2, (128, 512), BF16)
    TB = tbt * 128
    hxT = p.sb([128, 16, TB], BF16, "hxT")
    ysb = p.sb([128, 12, TB], BF16, "ysb")
    gbf = p.sb([128, 4, TB], BF16, "gbf")
    ydT = p.sb([128, 4, TB], BF16, "ydT")
    mT = p.sb([128, 16, TB], BF16, "mT")
    xs = p.sb([128, tbt, D], F32, "xs")
    hxb = p.sb([128, D], BF16, "hxb")
    wbuf = [p.sb([128, 16, 512], BF16, "wbuf") for _ in range(2)]
    wbb = [p.sb([128, 16, 128], BF16, "wbb") for _ in range(2)]
    wglu_sb = p.sb([128, 4, 512], BF16, "wglu")
    wr_sb = p.sb([128, 16, 16], F32, "wr")
    gm1, sh1, md2, gm2, sh2 = [p.sb([128, D], F32, "bc%d" % i) for i in range(5)]
    bct = p.sb([128, D], F32, "bct")
    sg = p.sb([128, TB], F32, "sg")
    t2 = p.sb([128, TB], F32, "t2")
    acc = p.sb([128, TB], F32, "acc")
    ot = p.sb([128, 512], F32, "ot")
    hx2f = p.sb([128, D], F32, "hx2f")
    hx2b = p.sb([128, D], BF16, "hx2b")
    hx2T = p.sb([128, 16, 128], F32, "hx2T")
    rl = p.sb([128, 16], F32, "rl")
    rmx = p.sb([128, 1], F32, "rmx")
    rsm = p.sb([128, 1], F32, "rsm")

    p.dma("pool", wglu_sb[:], wglu[:, :].rearrange("(k p) c -> p k c", p=128), reads=[wglu], writes=[wglu_sb])
    p.dma("sp", wr_sb[:], wr[:, :].rearrange("(k p) c -> p k c", p=128), reads=[wr], writes=[wr_sb])

    cur = {"k": None}

    def ensure_kind(kind):
        if cur["k"] == kind:
            return
        cur["k"] = kind
        md = modd[kind]
        load_gmul_shift(p, g1, md, 0, 1, gm1, sh1, bct)
        load_gmul_shift(p, g2, md, 3, 4, gm2, sh2, bct)
        p.dma("sp", md2[:], md[2:3, :].partition_broadcast(128), reads=[md], writes=[md2])

    wi_ = [0]

    def do_block(b0):
        wi = wi_[0]
        tiles = list(range(b0, min(b0 + tbt, nt)))
        nb = len(tiles)
        tb = nb * 128
        t0 = b0 * 128
        for ti, tg in enumerate(tiles):
            ensure_kind(kinds[tg])
            p.dma("sp", xs[:, ti, :], xin[tg * 128:(tg + 1) * 128, :], reads=[xin], writes=[xs])
            ln.run(xs, xs[:, ti, :], gm1, sh1, hxb, hxb[:])
            transpose_to(p, hxb, lambda k: hxb[:, k * 128:(k + 1) * 128], 16, idb, ppt, hxT,
                         lambda k0, n, ti=ti: hxT[:, k0:k0 + n, ti * 128:(ti + 1) * 128])
        p.dma("pool", ysb[:, :, 0:tb], yT[0:1536, t0:t0 + tb].rearrange("(k p) t -> p k t", p=128),
              reads=[yT], writes=[ysb])
        p.dma("pool", gbf[:, :, 0:tb], yT[1536:2048, t0:t0 + tb].rearrange("(k p) t -> p k t", p=128),
              reads=[yT], writes=[gbf])
        for j in range(4):
            ps = pp.next()
            for i in range(4):
                p.mm(ps, ps[:, 0:tb], wglu_sb, wglu_sb[:, i, j * 128:(j + 1) * 128], gbf, gbf[:, i, 0:tb],
                     start=(i == 0), stop=(i == 3))
            p.op("act", lambda e, ps=ps: e.activation(out=sg[:, 0:tb], in_=ps[:, 0:tb], func=AF.Sigmoid),
                 reads=[ps], writes=[sg])
            p.op("dve", lambda e, j=j: e.tensor_tensor(out=ydT[:, j, 0:tb], in0=sg[:, 0:tb], in1=gbf[:, j, 0:tb],
                                                       op=ALU.mult), reads=[sg, gbf], writes=[ydT])
        for dc in range(16):
            wgb, wb = wbuf[wi % 2], wbb[wi % 2]
            wi += 1
            p.dma("pool", wgb[:], wg[dc].rearrange("(k p) c -> p k c", p=128), reads=[wg], writes=[wgb])
            p.dma("pool", wb[:], wbr[dc].rearrange("(k p) c -> p k c", p=128), reads=[wbr], writes=[wb])
            for n in range(4):
                psg = pp.next()
                for k in range(16):
                    p.mm(psg, psg[:, 0:tb], wgb, wgb[:, k, n * 128:(n + 1) * 128], hxT, hxT[:, k, 0:tb],
                         start=(k == 0), stop=(k == 15))
                psp = pp.next()
                for i in range(4):
                    if n < 3:
                        rb, rap = ysb, ysb[:, n * 4 + i, 0:tb]
                    else:
                        rb, rap = ydT, ydT[:, i, 0:tb]
                    p.mm(psp, psp[:, 0:tb], wb, wb[:, n * 4 + i, :], rb, rap, start=(i == 0), stop=(i == 3))
                p.op("act", lambda e, psg=psg: e.activation(out=sg[:, 0:tb], in_=psg[:, 0:tb], func=AF.Sigmoid),
                     reads=[psg], writes=[sg])
                if n == 0:
                    p.op("dve", lambda e, psp=psp: e.tensor_tensor(out=acc[:, 0:tb], in0=sg[:, 0:tb],
                                                                   in1=psp[:, 0:tb], op=ALU.mult),
                         reads=[sg, psp], writes=[acc])
                else:
                    p.op("dve", lambda e, psp=psp: e.tensor_tensor(out=t2[:, 0:tb], in0=sg[:, 0:tb],
                                                                   in1=psp[:, 0:tb], op=ALU.mult),
                         reads=[sg, psp], writes=[t2])
                    if n < 3:
                        p.op("pool", lambda e: e.tensor_tensor(out=acc[:, 0:tb], in0=acc[:, 0:tb], in1=t2[:, 0:tb],
                                                               op=ALU.add), reads=[acc, t2], writes=[acc])
                    else:
                        p.op("pool", lambda e, dc=dc: e.tensor_tensor(out=mT[:, dc, 0:tb], in0=acc[:, 0:tb],
                                                                      in1=t2[:, 0:tb], op=ALU.add),
                             reads=[acc, t2], writes=[mT])
        for dq in range(4):
            wq = wbuf[wi % 2]
            wi += 1
            p.dma("pool", wq[:], wout[:, dq * 512:(dq + 1) * 512].rearrange("(k p) c -> p k c", p=128),
                  reads=[wout], writes=[wq])
            for ti, tg in enumerate(tiles):
                ensure_kind(kinds[tg])
                pso = pp.next()
                for k in range(16):
                    p.mm(pso, pso[:], mT, mT[:, k, ti * 128:(ti + 1) * 128], wq, wq[:, k, :],
                         start=(k == 0), stop=(k == 15))
                cs = slice(dq * 512, (dq + 1) * 512)
                p.op("dve", lambda e, pso=pso, cs=cs: e.tensor_tensor(out=ot[:], in0=pso[:], in1=md2[:, cs],
                                                                      op=ALU.mult),
                     reads=[pso, md2], writes=[ot])
                p.op("pool", lambda e, ti=ti, cs=cs: e.tensor_tensor(out=xs[:, ti, cs], in0=xs[:, ti, cs],
                                                                     in1=ot[:], op=ALU.add),
                     reads=[xs, ot], writes=[xs])
        for ti, tg in enumerate(tiles):
            ensure_kind(kinds[tg])
            rows = slice(tg * 128, (tg + 1) * 128)
            p.dma("sp", xnew[rows, :], xs[:, ti, :], reads=[xs], writes=[xnew])
            ln.run(xs, xs[:, ti, :], gm2, sh2, hx2f, hx2f[:])
            p.op("act", lambda e: e.copy(out=hx2b[:], in_=hx2f[:]), reads=[hx2f], writes=[hx2b])
            p.dma("sp", hx2o[rows, :], hx2b[:], reads=[hx2b], writes=[hx2o])
            for k0 in range(0, 16, 4):
                pst = pp.next()
                for j in range(4):
                    p.tr(pst, pst[:, j * 128:(j + 1) * 128], hx2f, hx2f[:, (k0 + j) * 128:(k0 + j + 1) * 128],
                         idf, idf[:])
                p.op("act", lambda e, pst=pst, k0=k0: e.copy(
                    out=hx2T[:, k0:k0 + 4, :], in_=pst[:].rearrange("p (k t) -> p k t", t=128)),
                    reads=[pst], writes=[hx2T])
            psr = pp.next()
            for k in range(16):
                p.mm(psr, psr[:, 0:16], hx2T, hx2T[:, k, :], wr_sb, wr_sb[:, k, :], start=(k == 0), stop=(k == 15))
            p.op("dve", lambda e, psr=psr: e.reduce_max(out=rmx[:], in_=psr[:, 0:16], axis=AX.X),
                 reads=[psr], writes=[rmx])
            p.op("dve", lambda e: e.tensor_scalar(out=rmx[:], in0=rmx[:], scalar1=-1.0, scalar2=None, op0=ALU.mult),
                 reads=[rmx], writes=[rmx])
            p.op("act", lambda e, psr=psr: e.activation(out=rl[:], in_=psr[:, 0:16], func=AF.Exp, bias=rmx[:, 0:1]),
                 reads=[psr, rmx], writes=[rl])
            p.op("dve", lambda e: e.reduce_sum(out=rsm[:], in_=rl[:], axis=AX.X), reads=[rl], writes=[rsm])
            p.op("dve", lambda e: e.reciprocal(out=rsm[:], in_=rsm[:]), reads=[rsm], writes=[rsm])
            p.op("dve", lambda e: e.tensor_scalar(out=rl[:], in0=rl[:], scalar1=rsm[:, 0:1], scalar2=None,
                                                  op0=ALU.mult), reads=[rl, rsm], writes=[rl])
            p.dma("sp", affo[rows, :], rl[:], reads=[rl], writes=[affo])
        wi_[0] = wi

    for b0_ in range(0, nt, tbt):
        do_block(b0_)
    return p


NCTX = 256
NLAT = 2048
NTOK = NCTX + NLAT
B1_CHUNKS = [(0, True), None, (1, True), None,
             (2, True), None, (3, True), None,
             (4, True), None, (5, True), None,
             (6, True), None, (7, False), (8, False),
             (9, False), (10, False), (11, False), (12, False)]
TOKCH = [(0, 256)] + [(256 + i * 512, 512) for i in range(4)]


def build_proj():
    p = Prog()
    xin = p.dram("xin", [NTOK, D], F32, "ExternalInput")
    modd = {"x": p.dram("modx", [6, D], F32, "ExternalInput"), "c": p.dram("modc", [6, D], F32, "ExternalInput")}
    g1 = p.dram("g1", [1, D], F32, "ExternalInput")
    wsel = p.dram("wsel", [D, 3136], F32, "ExternalInput")
    cosd = p.dram("cos", [128, NLAT], F32, "ExternalInput")
    sind = p.dram("sin", [128, NLAT], F32, "ExternalInput")
    ident = p.dram("ident", [128, 128], F32, "ExternalInput")
    qko = p.dram("qkT", [13, 128, NTOK], BF16, "ExternalOutput")
    vo = p.dram("v", [NTOK, 576], BF16, "ExternalOutput")

    idf, idb = load_ident(p, ident)
    ln = LN(p)
    pp = PsPool(p, 4)
    ppt = PsPool(p, 2, (128, 512), BF16)
    hxT = p.sb([128, 16, NTOK], BF16, "hxT")
    xt = p.sb([128, D], F32, "xt")
    hxb = p.sb([128, D], BF16, "hxb")
    gm1 = p.sb([128, D], F32, "gm1")
    sh1 = p.sb([128, D], F32, "sh1")
    cos = p.sb([128, NLAT], F32, "cos")
    sin = p.sb([128, NLAT], F32, "sin")
    wb = [p.sb([128, 16, 512], BF16, "w") for _ in range(2)]
    t1 = p.sb([128, 512], F32, "t1")
    t2 = p.sb([128, 512], F32, "t2")
    och = [p.sb([128, NTOK], BF16, "och") for _ in range(2)]
    vsb = p.sb([128, NTOK // 128, 576], BF16, "vsb")
    p.dma("sp", cos[:], cosd[:, :], reads=[cosd], writes=[cos])
    p.dma("sp", sin[:], sind[:, :], reads=[sind], writes=[sin])

    for tt in range(NTOK // 128):
        kind = "c" if tt < NCTX // 128 else "x"
        if tt == 0 or tt == NCTX // 128:
            load_gmul_shift(p, g1, modd[kind], 0, 1, gm1, sh1, ln.junk)
        p.dma("sp", xt[:], xin[tt * 128:(tt + 1) * 128, :], reads=[xin], writes=[xt])
        ln.run(xt, xt[:], gm1, sh1, hxb, hxb[:])
        transpose_to(p, hxb, lambda k: hxb[:, k * 128:(k + 1) * 128], 16, idb, ppt, hxT,
                     lambda k0, n, tt=tt: hxT[:, k0:k0 + n, tt * 128:(tt + 1) * 128])

    oi = 0
    for g in range(5):
        w = wb[g % 2]
        p.dma("pool", w[:], wsel[:, g * 512:(g + 1) * 512].rearrange("(k p) c -> p k c", p=128),
              reads=[wsel], writes=[w])
        for j in range(4):
            ent = B1_CHUNKS[g * 4 + j]
            if ent is None:
                continue
            cid, rope = ent
            ob = och[oi % 2]
            oi += 1
            for (t0, tn) in TOKCH:
                psa = pp.next()
                for k in range(16):
                    p.mm(psa, psa[:, 0:tn], w, w[:, k, j * 128:(j + 1) * 128], hxT, hxT[:, k, t0:t0 + tn],
                         start=(k == 0), stop=(k == 15))
                if rope and t0 >= NCTX:
                    psb = pp.next()
                    for k in range(16):
                        p.mm(psb, psb[:, 0:tn], w, w[:, k, (j + 1) * 128:(j + 2) * 128], hxT, hxT[:, k, t0:t0 + tn],
                             start=(k == 0), stop=(k == 15))
                    l0 = t0 - NCTX
                    p.op("dve", lambda e, psa=psa, l0=l0, tn=tn: e.tensor_tensor(
                        out=t1[:, 0:tn], in0=psa[:, 0:tn], in1=cos[:, l0:l0 + tn], op=ALU.mult),
                        reads=[psa, cos], writes=[t1])
                    p.op("dve", lambda e, psb=psb, l0=l0, tn=tn: e.tensor_tensor(
                        out=t2[:, 0:tn], in0=psb[:, 0:tn], in1=sin[:, l0:l0 + tn], op=ALU.mult),
                        reads=[psb, sin], writes=[t2])
                    p.op("pool", lambda e, ob=ob, t0=t0, tn=tn: e.tensor_tensor(
                        out=ob[:, t0:t0 + tn], in0=t1[:, 0:tn], in1=t2[:, 0:tn], op=ALU.add),
                        reads=[t1, t2], writes=[ob])
                else:
                    p.op("act", lambda e, ob=ob, psa=psa, t0=t0, tn=tn: e.copy(out=ob[:, t0:t0 + tn], in_=psa[:, 0:tn]),
                         reads=[psa], writes=[ob])
            p.dma("sp", qko[cid], ob[:], reads=[ob], writes=[qko])

    for (c0, cn, wi) in ((2560, 512, 1), (3072, 64, 0)):
        w = wb[wi]
        p.dma("pool", w[:, :, 0:cn], wsel[:, c0:c0 + cn].rearrange("(k p) c -> p k c", p=128),
              reads=[wsel], writes=[w])
        for tt in range(NTOK // 128):
            ps = pp.next()
            for k in range(16):
                p.mm(ps, ps[:, 0:cn], hxT, hxT[:, k, tt * 128:(tt + 1) * 128], w, w[:, k, 0:cn],
                     start=(k == 0), stop=(k == 15))
            vc0 = c0 - 2560
            p.op("act", lambda e, ps=ps, tt=tt, vc0=vc0, cn=cn: e.copy(out=vsb[:, tt, vc0:vc0 + cn], in_=ps[:, 0:cn]),
                 reads=[ps], writes=[vsb])
    p.dma("sp", vo[:, :].rearrange("(t p) c -> p t c", p=128), vsb[:], reads=[vsb], writes=[vo])
    return p


def _rope_partner(j):
    jj = j % 32
    return j + 16 if jj < 16 else j - 16


def proj_cols(h):
    off = np.cumsum([0, 512, 512, 512, 512, 128, 128, 512, 512, 512, 512])
    o_qa, o_ka, o_va, o_qb, o_kb, o_vb, o_qc, o_kc, o_vc, o_u = off[:10]
    sw = np.array([_rope_partner(j) for j in range(64)])
    cols = []

    def chunk(base_list, swapped):
        for base in base_list:
            idx = base + (sw if swapped else np.arange(64))
            cols.extend(idx.tolist())

    for hd in (2 * h, 2 * h + 1):
        qb = [o_qa + hd * 128, o_qa + hd * 128 + 64]
        kb = [o_ka + hd * 128, o_ka + hd * 128 + 64]
        chunk(qb, False); chunk(qb, True); chunk(kb, False); chunk(kb, True)
    for pr in (0, 1):
        qb = [o_qb + (h * 4 + 2 * pr) * 64, o_qb + (h * 4 + 2 * pr + 1) * 64]
        chunk(qb, False); chunk(qb, True)
    kb = [o_kb + h * 64, o_kb + h * 64]
    chunk(kb, False); chunk(kb, True)
    for pr in (0, 1):
        chunk([o_qc + (h * 4 + 2 * pr) * 64, o_qc + (h * 4 + 2 * pr + 1) * 64], False)
    for pr in (0, 1):
        chunk([o_kc + (h * 4 + 2 * pr) * 64, o_kc + (h * 4 + 2 * pr + 1) * 64], False)
    cols.extend(range(o_u + h * 256, o_u + (h + 1) * 256))
    cols.extend(range(o_va + 2 * h * 128, o_va + (2 * h + 2) * 128))
    cols.extend(range(o_vc + h * 256, o_vc + (h + 1) * 256))
    cols.extend(range(o_vb + h * 64, o_vb + (h + 1) * 64))
    return np.array(cols)


def rope_tables():
    pos = np.arange(NLAT)
    rows, colsp = (pos // 64).astype(np.float32), (pos % 64).astype(np.float32)
    inv = np.power(np.float32(10000.0), -np.arange(0, 32, 2, dtype=np.float32) / np.float32(32)).astype(np.float32)
    cos = np.zeros((64, NLAT), np.float32)
    sin = np.zeros((64, NLAT), np.float32)
    for j in range(64):
        pp_ = rows if j < 32 else colsp
        jj = j % 32
        ang = (pp_ * inv[jj % 16]).astype(np.float32)
        cos[j] = np.cos(ang)
        sin[j] = -np.sin(ang) if jj < 16 else np.sin(ang)
    return np.ascontiguousarray(np.tile(cos, (2, 1))), np.ascontiguousarray(np.tile(sin, (2, 1)))


ATT_SCALE = 0.125
NTT = NTOK // 128


def na_blocks(i):
    lo = min(max(2 * i - 4, 0), 24)
    return [lo // 2 + j for j in range(5) if lo // 2 + j <= 15]


def na_tables():
    uniq, idx, tiles = {}, {}, []
    q = np.arange(128)
    qr, wq = q // 64, q % 64
    cs = np.clip(wq - 8, 0, 48)
    for i in range(16):
        for kb in na_blocks(i):
            kr = 2 * kb + qr[:, None]
            wk = wq[:, None]
            r = 2 * i + qr[None, :]
            rs = np.clip(r - 4, 0, 24)
            ok = (kr >= rs) & (kr < rs + 8) & (wk >= cs[None, :]) & (wk < cs[None, :] + 16)
            m = ok.astype(np.float32)
            key = m.tobytes()
            if key not in uniq:
                uniq[key] = len(tiles)
                tiles.append(m)
            idx[(i, kb)] = uniq[key]
    return np.stack(tiles), idx


def na_rpb_gather(rpb4):
    q = np.arange(128)
    qr, wq = q // 64, q % 64
    out = np.zeros((8, 128, 4, 128), np.float32)
    dc = np.clip(wq[:, None] - wq[None, :], -15, 15) + 15
    for d in range(-3, 5):
        dr = np.clip(2 * d + qr[:, None] - qr[None, :] + 7, 0, 14)
        for j in range(4):
            out[d + 3, :, j, :] = rpb4[j][dr, dc]
    return np.ascontiguousarray(out.reshape(8, 128, 512))


def wb_masks():
    k = np.arange(128)[:, None]
    q = np.arange(128)[None, :]
    prev = (k >= q).astype(np.float32)
    nxt = (k <= q).astype(np.float32)
    return np.ascontiguousarray(np.stack([np.tile(prev, (1, 4)), np.tile(nxt, (1, 4))]))


def build_attn(lam_init, na_idx, n_nam, parts=("da", "wb", "na"), tts=None):
    p = Prog()
    qkd = p.dram("qkT", [13, 128, NTOK], BF16, "ExternalInput")
    vd = p.dram("v", [NTOK, 576], BF16, "ExternalInput")
    lamd = p.dram("lamb", [1, 256], F32, "ExternalInput")
    gsd = p.dram("gsub", [1, 128], F32, "ExternalInput")
    skd = p.dram("sink", [1, 4], F32, "ExternalInput")
    wbmd = p.dram("wbm", [2, 128, 512], F32, "ExternalInput")
    namd = p.dram("nam", [n_nam, 128, 512], F32, "ExternalInput")
    rpbd = p.dram("rpbT", [8, 128, 512], F32, "ExternalInput")
    outs = [p.dram(n, [NTOK, 256], F32, "ExternalOutput") for n in ("yda", "ywb", "yna")]

    QCH = (0, 2, 4, 5, 7, 8)
    qk = {}
    qm = {}
    for c in range(11):
        if c in QCH:
            qm[c] = [p.sb([128, NTOK], BF16, "qm%d_%d" % (c, m)) for m in range(2)]
            for m in range(2):
                z = slice((1 - m) * 64, (2 - m) * 64)
                r = slice(m * 64, (m + 1) * 64)
                p.op("pool", lambda e, c=c, m=m, z=z: e.memset(qm[c][m][z, :], 0.0), writes=[qm[c][m]])
                p.dma("sp", qm[c][m][r, :], qkd[c, r, :], reads=[qkd], writes=[qm[c][m]])
        else:
            qk[c] = p.sb([128, NTOK], BF16, "qk%d" % c)
            p.dma("sp", qk[c][:], qkd[c], reads=[qkd], writes=[qk[c]])
    vda = p.sb([128, NTT, 2, 129], BF16, "vda")
    vna = p.sb([128, NTT, 4, 65], BF16, "vna")
    vwb = p.sb([128, NTT, 65], BF16, "vwb")
    for (buf, ap) in ((vda, vda[:, :, :, 128:129]), (vna, vna[:, :, :, 64:65]), (vwb, vwb[:, :, 64:65])):
        p.op("pool", lambda e, ap=ap: e.memset(ap, 1.0), writes=[buf])
    vv = vd[:, :].rearrange("(t p) c -> p t c", p=128)
    for g in range(2):
        p.dma("sp", vda[:, :, g, 0:128], vv[:, :, g * 128:(g + 1) * 128], reads=[vd], writes=[vda])
    for g in range(4):
        p.dma("sp", vna[:, :, g, 0:64], vv[:, :, 256 + g * 64:256 + (g + 1) * 64], reads=[vd], writes=[vna])
    p.dma("sp", vwb[:, :, 0:64], vv[:, :, 512:576], reads=[vd], writes=[vwb])
    wbm = p.sb([128, 2, 512], BF16, "wbm")
    nam = p.sb([128, n_nam, 512], BF16, "nam")
    rpb = p.sb([128, 8, 512], F32, "rpb")
    p.dma("pool", wbm[:], wbmd[:, :, :].rearrange("m p c -> p m c"), reads=[wbmd], writes=[wbm])
    p.dma("pool", nam[:], namd[:, :, :].rearrange("m p c -> p m c"), reads=[namd], writes=[nam])
    p.dma("sp", rpb[:], rpbd[:, :, :].rearrange("m p c -> p m c"), reads=[rpbd], writes=[rpb])

    pps = PsPool(p, 3)
    ppo = PsPool(p, 4)
    for b_ in ppo.t:
        b_.t = b_.t[:, 0:258].rearrange("p (s c) -> p s c", s=2)
    ysb = [p.sb([128, 256], F32, "ysb") for _ in range(2)]
    yi = [0]
    sm = {n: p.sb([128, 8], F32, n) for n in ("r0", "r1", "ss", "rstd", "den")}
    o0 = p.sb([128, 128], F32, "o0")
    o1 = p.sb([128, 128], F32, "o1")
    jk = p.sb([128, 128], F32, "jk")

    lv = p.sb([128, 256], F32, "lv")
    lp = p.sb([128, 128], F32, "lp")
    ls = p.sb([128, 2], F32, "ls")
    nlam = p.sb([128, 1], F32, "nlam")
    gsb = p.sb([128, 128], F32, "gsb")
    esk = p.sb([128, 4], F32, "esk")
    p.dma("sp", lv[:], lamd[0:1, :].partition_broadcast(128), reads=[lamd], writes=[lv])
    p.dma("sp", gsb[:], gsd[0:1, :].partition_broadcast(128), reads=[gsd], writes=[gsb])
    p.dma("sp", esk[:], skd[0:1, :].partition_broadcast(128), reads=[skd], writes=[esk])
    p.op("dve", lambda e: e.tensor_tensor(out=lp[:].rearrange("p (a c) -> p a c", a=2),
                                          in0=lv[:].rearrange("p (a b c) -> p a b c", a=2, b=2)[:, :, 0, :],
                                          in1=lv[:].rearrange("p (a b c) -> p a b c", a=2, b=2)[:, :, 1, :],
                                          op=ALU.mult), reads=[lv], writes=[lp])
    p.op("dve", lambda e: e.reduce_sum(out=ls[:], in_=lp[:].rearrange("p (a c) -> p a c", a=2), axis=AX.X),
         reads=[lp], writes=[ls])
    p.op("act", lambda e: e.activation(out=ls[:], in_=ls[:], func=AF.Exp), reads=[ls], writes=[ls])
    p.op("dve", lambda e: e.scalar_tensor_tensor(out=nlam[:], in0=ls[:, 1:2], scalar=-float(lam_init), in1=ls[:, 0:1],
                                                 op0=ALU.add, op1=ALU.subtract), reads=[ls], writes=[nlam])
    p.op("dve", lambda e: e.tensor_scalar(out=gsb[:], in0=gsb[:], scalar1=float(1.0 - lam_init), scalar2=None,
                                          op0=ALU.mult), reads=[gsb], writes=[gsb])
    p.op("act", lambda e: e.activation(out=esk[:], in_=esk[:], func=AF.Exp), reads=[esk], writes=[esk])

    def next_y():
        y = ysb[yi[0] % 2]
        yi[0] += 1
        return y

    Eda = [p.sb([128, NTT, 512], BF16, "Eda%d" % m) for m in range(2)]
    for i in (range(2) if "da" in parts else ()):
        kT = qk[2 * i + 1]
        for (q0, qn) in TOKCH:
            kbs = [0, 1] if q0 < NCTX else list(range(NTT))
            for m in range(2):
                qT = qm[2 * i][m]
                for kb in kbs:
                    ps = pps.next()
                    p.mm(ps, ps[:, 0:qn], kT, kT[:, kb * 128:(kb + 1) * 128], qT, qT[:, q0:q0 + qn])
                    p.op("act", lambda e, ps=ps, m=m, kb=kb, qn=qn: e.activation(
                        out=Eda[m][:, kb, 0:qn], in_=ps[:, 0:qn], func=AF.Exp, scale=ATT_SCALE),
                        reads=[ps], writes=[Eda[m]])
            for qp in range(0, qn // 128, 2):
                po = [ppo.next(), ppo.next()]
                for m in range(2):
                    for s in range(2):
                        qc = slice((qp + s) * 128, (qp + s + 1) * 128)
                        for n, kb in enumerate(kbs):
                            p.mm(po[m], po[m][:, s, :], Eda[m], Eda[m][:, kb, qc], vda, vda[:, kb, i, :],
                                 start=(n == 0), stop=(n == len(kbs) - 1))
                for s in range(2):
                    tt = q0 // 128 + qp + s
                    r0, r1, ss, rstd = sm["r0"], sm["r1"], sm["ss"], sm["rstd"]
                    p.op("dve", lambda e, po=po, s=s: e.reciprocal(out=r0[:, 0:1], in_=po[0][:, s, 128:129]),
                         reads=[po[0]], writes=[r0])
                    p.op("dve", lambda e, po=po, s=s: e.reciprocal(out=r1[:, 0:1], in_=po[1][:, s, 128:129]),
                         reads=[po[1]], writes=[r1])
                    p.op("dve", lambda e: e.tensor_scalar(out=r1[:, 0:1], in0=r1[:, 0:1], scalar1=nlam[:, 0:1],
                                                          scalar2=None, op0=ALU.mult), reads=[r1, nlam], writes=[r1])
                    p.op("act", lambda e, po=po, s=s: e.activation(out=o0[:], in_=po[0][:, s, 0:128], func=AF.Copy,
                                                                   scale=r0[:, 0:1]), reads=[po[0], r0], writes=[o0])
                    p.op("dve", lambda e, po=po, s=s: e.scalar_tensor_tensor(
                        out=o1[:], in0=po[1][:, s, 0:128], scalar=r1[:, 0:1], in1=o0[:], op0=ALU.mult, op1=ALU.add),
                        reads=[po[1], r1, o0], writes=[o1])
                    p.op("act", lambda e: e.activation(out=jk[:], in_=o1[:], func=AF.Square), reads=[o1], writes=[jk])
                    p.op("dve", lambda e: e.reduce_sum(out=ss[:, 0:1], in_=jk[:], axis=AX.X), reads=[jk], writes=[ss])
                    p.op("act", lambda e: e.activation(out=rstd[:, 0:1], in_=ss[:, 0:1], func=AF.Sqrt, scale=1.0 / 128,
                                                       bias=EPS), reads=[ss], writes=[rstd])
                    p.op("dve", lambda e: e.reciprocal(out=rstd[:, 0:1], in_=rstd[:, 0:1]), reads=[rstd], writes=[rstd])
                    y = next_y()
                    p.op("dve", lambda e, y=y: e.scalar_tensor_tensor(
                        out=y[:, 0:128], in0=o1[:], scalar=rstd[:, 0:1], in1=gsb[:], op0=ALU.mult, op1=ALU.mult),
                        reads=[o1, rstd, gsb], writes=[y])
                    p.dma("sp", outs[0][tt * 128:(tt + 1) * 128, i * 128:(i + 1) * 128], y[:, 0:128],
                          reads=[y], writes=[outs[0]])

    Ebn = [p.sb([128, 7, 512], BF16, "Ebn%d" % i) for i in range(2)]
    tmp = [p.sb([128, 512], F32, "tmpb%d" % i) for i in range(2)]
    cnt = [0]

    def grouped(mixer, tt, heads, kT_of, blocks, vbuf, vslice, outd, sink):
        E = Ebn[cnt[0] % 2]
        cnt[0] += 1
        qc = slice(tt * 128, (tt + 1) * 128)
        for n, (kb, mask, bias) in enumerate(blocks):
            ps = pps.next()
            for g, qb in enumerate(heads):
                kT = kT_of(g)
                p.mm(ps, ps[:, g * 128:(g + 1) * 128], kT, kT[:, kb * 128:(kb + 1) * 128], qb, qb[:, qc])
            if bias is None:
                p.op("act", lambda e, ps=ps, n=n, E=E: e.activation(out=E[:, n, :], in_=ps[:], func=AF.Exp,
                                                                    scale=ATT_SCALE), reads=[ps], writes=[E])
            else:
                tb = tmp[n % 2]
                p.op("dve", lambda e, ps=ps, tb=tb, bias=bias: e.scalar_tensor_tensor(
                    out=tb[:], in0=ps[:], scalar=ATT_SCALE, in1=bias, op0=ALU.mult, op1=ALU.add),
                    reads=[ps, rpb], writes=[tb])
                p.op("act", lambda e, tb=tb, n=n, E=E: e.activation(out=E[:, n, :], in_=tb[:], func=AF.Exp),
                     reads=[tb], writes=[E])
            if mask is not None:
                p.op("pool", lambda e, n=n, E=E, mask=mask: e.tensor_tensor(out=E[:, n, :], in0=E[:, n, :], in1=mask,
                                                                            op=ALU.mult), reads=[E, wbm, nam], writes=[E])
        po2 = [ppo.next(), ppo.next()]
        for g in range(4):
            for n, (kb, _, _) in enumerate(blocks):
                p.mm(po2[g // 2], po2[g // 2][:, g % 2, 0:65], E, E[:, n, g * 128:(g + 1) * 128], vbuf, vslice(kb, g),
                     start=(n == 0), stop=(n == len(blocks) - 1))
        den = sm["den"]
        y = next_y()
        for g in range(4):
            po_ = po2[g // 2]
            if sink:
                p.op("dve", lambda e, po_=po_, g=g: e.tensor_tensor(out=den[:, g:g + 1], in0=po_[:, g % 2, 64:65],
                                                                    in1=esk[:, g:g + 1], op=ALU.add),
                     reads=[po_, esk], writes=[den])
                p.op("dve", lambda e, g=g: e.reciprocal(out=den[:, g:g + 1], in_=den[:, g:g + 1]),
                     reads=[den], writes=[den])
            else:
                p.op("dve", lambda e, po_=po_, g=g: e.reciprocal(out=den[:, g:g + 1], in_=po_[:, g % 2, 64:65]),
                     reads=[po_], writes=[den])
            p.op("act", lambda e, po_=po_, g=g, y=y: e.activation(out=y[:, g * 64:(g + 1) * 64], in_=po_[:, g % 2, 0:64],
                                                                  func=AF.Copy, scale=den[:, g:g + 1]),
                 reads=[po_, den], writes=[y])
        p.dma("sp", outd[tt * 128:(tt + 1) * 128, :], y[:], reads=[y], writes=[outd])

    wb_heads = [qm[4 + g // 2][g % 2] for g in range(4)]
    na_heads = [qm[7 + g // 2][g % 2] for g in range(4)]
    for tt in (range(NTT) if tts is None else tts):
        blocks = [(0, None, None), (1, None, None)]
        if tt >= 2:
            i = tt - 2
            if i > 0:
                blocks.append((tt - 1, wbm[:, 0, :], None))
            blocks.append((tt, None, None))
            if i < 15:
                blocks.append((tt + 1, wbm[:, 1, :], None))
        if "wb" in parts:
            grouped("wb", tt, wb_heads, lambda g: qk[6], blocks, vwb, lambda kb, g: vwb[:, kb, :], outs[1], True)
        blocks = [(0, None, None), (1, None, None)]
        if tt >= 2:
            i = tt - 2
            for kb in na_blocks(i):
                blocks.append((2 + kb, nam[:, na_idx[(i, kb)], :], rpb[:, kb - i + 3, :]))
        if "na" in parts:
            grouped("na", tt, na_heads, lambda g: qk[9 + g // 2], blocks, vna, lambda kb, g: vna[:, kb, g, :], outs[2], False)
    return p


NSTEP = 12


def build_s5():
    p = Prog()
    ud = p.dram("uT", [2, 128, NTOK], BF16, "ExternalInput")
    bd = [p.dram(n, [2, 8, 128, 128], F32, "ExternalInput") for n in ("bR", "bI")]
    cd = [p.dram(n, [2, 8, 128, 128], F32, "ExternalInput") for n in ("cR", "cI")]
    apd = p.dram("apar", [3, 128, 16], F32, "ExternalInput")
    dd = p.dram("dsk", [128, 2], F32, "ExternalInput")
    go = p.dram("gT", [2, 128, NTOK], F32, "ExternalOutput")
    T = NTOK
    u = [p.sb([128, T], BF16, "u%d" % c) for c in range(2)]
    for c in range(2):
        p.dma("sp", u[c][:], ud[c], reads=[ud], writes=[u[c]])
    Bm = [p.sb([128, 16, 128], BF16, "B%d" % i) for i in range(2)]
    Cm = [p.sb([128, 16, 128], F32, "C%d" % i) for i in range(2)]
    for i in range(2):
        p.dma("pool", Bm[i][:], bd[i][:, :, :, :].rearrange("d j c s -> c (d j) s"), reads=[bd[i]], writes=[Bm[i]])
        p.dma("sp", Cm[i][:], cd[i][:, :, :, :].rearrange("d j s c -> s (d j) c"), reads=[cd[i]], writes=[Cm[i]])
    ap_ = p.sb([128, 3, 16], F32, "apar")
    p.dma("sp", ap_[:], apd[:, :, :].rearrange("k p c -> p k c"), reads=[apd], writes=[ap_])
    dsk = p.sb([128, 2], F32, "dsk")
    p.dma("sp", dsk[:], dd[:, :], reads=[dd], writes=[dsk])

    names = ("dt", "x", "ang", "er", "s", "c", "ss", "cc", "sc", "are", "aim", "nr", "den", "t1", "t2", "cre", "cim",
             "ncre", "ncim")
    w = {n: p.sb([128, 16], F32, "w_" + n) for n in names}
    a_re, a_im, ls = ap_[:, 0, :], ap_[:, 1, :], ap_[:, 2, :]

    def tt(o, a, b, op, ra=(), eng="dve"):
        p.op(eng, lambda e: e.tensor_tensor(out=w[o][:], in0=a, in1=b, op=op), reads=list(ra) + [ap_], writes=[w[o]])

    def W(n):
        return w[n][:]

    p.op("act", lambda e: e.activation(out=W("dt"), in_=ls, func=AF.Exp), reads=[ap_], writes=[w["dt"]])
    tt("x", a_re, W("dt"), ALU.mult, [w["dt"]])
    tt("ang", a_im, W("dt"), ALU.mult, [w["dt"]])
    p.op("act", lambda e: e.activation(out=W("er"), in_=W("x"), func=AF.Exp), reads=[w["x"]], writes=[w["er"]])
    p.op("act", lambda e: e.activation(out=W("s"), in_=W("ang"), func=AF.Sin, scale=1.0 / 16), reads=[w["ang"]], writes=[w["s"]])
    p.op("act", lambda e: e.activation(out=W("c"), in_=W("ang"), func=AF.Sin, scale=1.0 / 16, bias=math.pi / 2),
         reads=[w["ang"]], writes=[w["c"]])
    for _ in range(4):
        tt("ss", W("s"), W("s"), ALU.mult, [w["s"]])
        tt("cc", W("c"), W("c"), ALU.mult, [w["c"]])
        tt("sc", W("s"), W("c"), ALU.mult, [w["s"], w["c"]])
        tt("c", W("cc"), W("ss"), ALU.subtract, [w["cc"], w["ss"]])
        p.op("dve", lambda e: e.tensor_scalar(out=W("s"), in0=W("sc"), scalar1=2.0, scalar2=None, op0=ALU.mult),
             reads=[w["sc"]], writes=[w["s"]])
    tt("are", W("er"), W("c"), ALU.mult, [w["er"], w["c"]])
    tt("aim", W("er"), W("s"), ALU.mult, [w["er"], w["s"]])
    p.op("dve", lambda e: e.tensor_scalar(out=W("nr"), in0=W("are"), scalar1=-1.0, scalar2=None, op0=ALU.add),
         reads=[w["are"]], writes=[w["nr"]])
    tt("t1", a_re, a_re, ALU.mult)
    tt("t2", a_im, a_im, ALU.mult)
    tt("den", W("t1"), W("t2"), ALU.add, [w["t1"], w["t2"]])
    p.op("dve", lambda e: e.reciprocal(out=W("den"), in_=W("den")), reads=[w["den"]], writes=[w["den"]])
    tt("t1", W("nr"), a_re, ALU.mult, [w["nr"]])
    tt("t2", W("aim"), a_im, ALU.mult, [w["aim"]])
    tt("cre", W("t1"), W("t2"), ALU.add, [w["t1"], w["t2"]])
    tt("cre", W("cre"), W("den"), ALU.mult, [w["cre"], w["den"]])
    tt("t1", W("aim"), a_re, ALU.mult, [w["aim"]])
    tt("t2", W("nr"), a_im, ALU.mult, [w["nr"]])
    tt("cim", W("t1"), W("t2"), ALU.subtract, [w["t1"], w["t2"]])
    tt("cim", W("cim"), W("den"), ALU.mult, [w["cim"], w["den"]])
    for (o, i_) in (("ncre", "cre"), ("ncim", "cim")):
        p.op("dve", lambda e, o=o, i_=i_: e.tensor_scalar(out=W(o), in0=W(i_), scalar1=-1.0, scalar2=None, op0=ALU.mult),
             reads=[w[i_]], writes=[w[o]])
    Ar = p.sb([128, NSTEP, 16], F32, "Ar")
    Ai = p.sb([128, NSTEP, 16], F32, "Ai")
    nAi = p.sb([128, NSTEP, 16], F32, "nAi")
    p.op("dve", lambda e: e.tensor_copy(out=Ar[:, 0, :], in_=W("are")), reads=[w["are"]], writes=[Ar])
    p.op("dve", lambda e: e.tensor_copy(out=Ai[:, 0, :], in_=W("aim")), reads=[w["aim"]], writes=[Ai])
    for k in range(1, NSTEP):
        tt("t1", Ar[:, k - 1, :], Ar[:, k - 1, :], ALU.mult, [Ar])
        tt("t2", Ai[:, k - 1, :], Ai[:, k - 1, :], ALU.mult, [Ai])
        tt("sc", Ar[:, k - 1, :], Ai[:, k - 1, :], ALU.mult, [Ar, Ai])
        p.op("dve", lambda e, k=k: e.tensor_tensor(out=Ar[:, k, :], in0=W("t1"), in1=W("t2"), op=ALU.subtract),
             reads=[w["t1"], w["t2"]], writes=[Ar])
        p.op("dve", lambda e, k=k: e.tensor_scalar(out=Ai[:, k, :], in0=W("sc"), scalar1=2.0, scalar2=None, op0=ALU.mult),
             reads=[w["sc"]], writes=[Ai])
    p.op("dve", lambda e: e.tensor_scalar(out=nAi[:], in0=Ai[:], scalar1=-1.0, scalar2=None, op0=ALU.mult),
         reads=[Ai], writes=[nAi])
    LR = p.sb([128, 16, 128], F32, "LR")
    LI = p.sb([128, 16, 128], F32, "LI")
    ctmp = p.sb([128, 128], F32, "ctmp")
    for col in range(16):
        for (L, s1, s2) in ((LR, "cre", "ncim"), (LI, "ncim", "ncre")):
            p.op("dve", lambda e, col=col, s1=s1: e.tensor_scalar(out=ctmp[:], in0=Cm[0][:, col, :],
                                                                  scalar1=w[s1][:, col:col + 1], scalar2=None, op0=ALU.mult),
                 reads=[Cm[0], w[s1]], writes=[ctmp])
            p.op("dve", lambda e, col=col, s2=s2, L=L: e.scalar_tensor_tensor(
                out=L[:, col, :], in0=Cm[1][:, col, :], scalar=w[s2][:, col:col + 1], in1=ctmp[:], op0=ALU.mult, op1=ALU.add),
                reads=[Cm[1], w[s2], ctmp], writes=[L])

    pp = PsPool(p, 6)
    Hr = [p.sb([128, T], F32, "Hr%d" % i) for i in range(2)]
    Hi = [p.sb([128, T], F32, "Hi%d" % i) for i in range(2)]
    tmd = p.sb([128, T], F32, "tmd")
    tmp_ = p.sb([128, T], F32, "tmp")
    yacc = [p.sb([128, T], F32, "yacc%d" % c) for c in range(2)]
    first = [True, True]
    for d in range(2):
        for j in range(8):
            col = d * 8 + j
            ch = j // 4
            for (t0, tn) in TOKCH:
                d0 = t0 if d == 0 else (t0 - NCTX if t0 >= NCTX else NLAT + t0)
                for (Bx, Hx) in ((Bm[0], Hr[0]), (Bm[1], Hi[0])):
                    ps = pp.next()
                    p.mm(ps, ps[:, 0:tn], Bx, Bx[:, col, :], u[ch], u[ch][:, t0:t0 + tn])
                    p.op("act", lambda e, ps=ps, Hx=Hx, d0=d0, tn=tn: e.copy(out=Hx[:, d0:d0 + tn], in_=ps[:, 0:tn]),
                         reads=[ps], writes=[Hx])
            cur = 0
            for k in range(NSTEP):
                s = 1 << k
                if s >= T:
                    break
                nw = 1 - cur
                if d == 0:
                    sh, ds, kp = slice(0, T - s), slice(s, T), slice(0, s)
                else:
                    sh, ds, kp = slice(s, T), slice(0, T - s), slice(T - s, T)
                ar, ai, nai = Ar[:, k, col:col + 1], Ai[:, k, col:col + 1], nAi[:, k, col:col + 1]
                cr, ci, nr_, ni_ = Hr[cur], Hi[cur], Hr[nw], Hi[nw]
                p.op("dve", lambda e, cr=cr, sh=sh, ds=ds, ar=ar: e.scalar_tensor_tensor(
                    out=tmd[:, ds], in0=cr[:, sh], scalar=ar, in1=cr[:, ds], op0=ALU.mult, op1=ALU.add),
                    reads=[cr, Ar], writes=[tmd])
                p.op("dve", lambda e, ci=ci, nr_=nr_, sh=sh, ds=ds, nai=nai: e.scalar_tensor_tensor(
                    out=nr_[:, ds], in0=ci[:, sh], scalar=nai, in1=tmd[:, ds], op0=ALU.mult, op1=ALU.add),
                    reads=[ci, nAi, tmd], writes=[nr_])
                p.op("dve", lambda e, ci=ci, sh=sh, ds=ds, ar=ar: e.scalar_tensor_tensor(
                    out=tmp_[:, ds], in0=ci[:, sh], scalar=ar, in1=ci[:, ds], op0=ALU.mult, op1=ALU.add),
                    reads=[ci, Ar], writes=[tmp_])
                p.op("dve", lambda e, cr=cr, ni_=ni_, sh=sh, ds=ds, ai=ai: e.scalar_tensor_tensor(
                    out=ni_[:, ds], in0=cr[:, sh], scalar=ai, in1=tmp_[:, ds], op0=ALU.mult, op1=ALU.add),
                    reads=[cr, Ai, tmp_], writes=[ni_])
                p.op("act", lambda e, cr=cr, nr_=nr_, kp=kp: e.copy(out=nr_[:, kp], in_=cr[:, kp]), reads=[cr], writes=[nr_])
                p.op("act", lambda e, ci=ci, ni_=ni_, kp=kp: e.copy(out=ni_[:, kp], in_=ci[:, kp]), reads=[ci], writes=[ni_])
                cur = nw
            for (t0, tn) in TOKCH:
                hc = t0 if d == 0 else (t0 - NCTX if t0 >= NCTX else NLAT + t0)
                ps = pp.next()
                p.mm(ps, ps[:, 0:tn], LR, LR[:, col, :], Hr[cur], Hr[cur][:, hc:hc + tn], start=True, stop=False)
                p.mm(ps, ps[:, 0:tn], LI, LI[:, col, :], Hi[cur], Hi[cur][:, hc:hc + tn], start=False, stop=True)
                ya = yacc[ch]
                if first[ch]:
                    p.op("act", lambda e, ps=ps, ya=ya, t0=t0, tn=tn: e.copy(out=ya[:, t0:t0 + tn], in_=ps[:, 0:tn]),
                         reads=[ps], writes=[ya])
                else:
                    p.op("dve", lambda e, ps=ps, ya=ya, t0=t0, tn=tn: e.tensor_tensor(
                        out=ya[:, t0:t0 + tn], in0=ps[:, 0:tn], in1=ya[:, t0:t0 + tn], op=ALU.add), reads=[ps, ya], writes=[ya])
            first[ch] = False
            assert cur == 0
    for c in range(2):
        v, t_, t2_ = yacc[c], tmd, tmp_
        p.op("dve", lambda e, c=c, v=v: e.scalar_tensor_tensor(out=v[:], in0=u[c][:], scalar=dsk[:, c:c + 1], in1=v[:],
                                                               op0=ALU.mult, op1=ALU.add), reads=[u[c], dsk, v], writes=[v])
        p.op("pool", lambda e, v=v: e.tensor_tensor(out=t_[:], in0=v[:], in1=v[:], op=ALU.mult), reads=[v], writes=[t_])
        p.op("dve", lambda e: e.tensor_scalar(out=t_[:], in0=t_[:], scalar1=0.044715, scalar2=1.0, op0=ALU.mult, op1=ALU.add),
             reads=[t_], writes=[t_])
        p.op("pool", lambda e, v=v: e.tensor_tensor(out=t_[:], in0=t_[:], in1=v[:], op=ALU.mult), reads=[t_, v], writes=[t_])
        p.op("act", lambda e: e.activation(out=t2_[:], in_=t_[:], func=AF.Tanh, scale=0.7978845608028654),
             reads=[t_], writes=[t2_])
        p.op("dve", lambda e, v=v: e.scalar_tensor_tensor(out=t2_[:], in0=t2_[:], scalar=1.0, in1=v[:], op0=ALU.add,
                                                          op1=ALU.mult), reads=[t2_, v], writes=[t2_])
        p.op("act", lambda e: e.activation(out=t_[:], in_=t2_[:], func=AF.Copy, scale=0.5), reads=[t2_], writes=[t_])
        p.dma("sp", go[c], t_[:], reads=[t_], writes=[go])
    return p


def s5_host(h, a_re, a_im, log_step, b_re, b_im, c_re, c_im, d_skip):
    bR = np.zeros((2, 8, 128, 128), np.float32); bI = np.zeros_like(bR)
    cR = np.zeros((2, 8, 128, 128), np.float32); cI = np.zeros_like(cR)
    apar = np.zeros((3, 128, 16), np.float32)
    for d in range(2):
        for j in range(8):
            for g2 in range(2):
                g = 16 * h + 2 * j + g2
                lg = (2 * j + g2) % 8
                st = slice(g2 * 64, (g2 + 1) * 64)
                chs = slice(lg * 16, (lg + 1) * 16)
                bR[d, j, chs, st] = b_re[d, g].T
                bI[d, j, chs, st] = b_im[d, g].T
                cR[d, j, st, chs] = c_re[d, g].T
                cI[d, j, st, chs] = c_im[d, g].T
                apar[0, st, d * 8 + j] = a_re[d, g]
                apar[1, st, d * 8 + j] = a_im[d, g]
                apar[2, st, d * 8 + j] = log_step[d, g]
    dsk = np.ascontiguousarray(d_skip[h * 256:(h + 1) * 256].reshape(2, 128).T)
    return {"bR": bR, "bI": bI, "cR": cR, "cI": cI, "apar": apar, "dsk": dsk}


def build_moe(sets, NT, nbis=30):
    p = Prog()
    ns = len(sets)
    P2 = 2 * ns
    LMAX = max(n for (_, n, _) in sets)
    hxd = p.dram("hxT", [D, NT], BF16, "ExternalInput")
    ard = p.dram("affR", [P2, LMAX], F32, "ExternalInput")
    capd = p.dram("capv", [P2, 1], F32, "ExternalInput")
    atd = p.dram("affT", [NT, 2], F32, "ExternalInput")
    wd = [p.dram(n, [2, D, D], F32, "ExternalInput") for n in ("w1", "w3", "w2")]
    thrd = p.dram("thr_scr", [1, P2], F32, "Internal")
    part = p.dram("part", [NT, D], F32, "ExternalOutput")

    ar = p.sb([P2, LMAX], F32, "ar")
    cmpb = p.sb([P2, LMAX], F32, "cmp")
    capv = p.sb([P2, 1], F32, "capv")
    sc = {n: p.sb([P2, 1], F32, n) for n in ("lo", "hi", "mid", "cnt", "ge", "dl")}
    p.dma("sp", ar[:], ard[:, :], reads=[ard], writes=[ar])
    p.dma("sp", capv[:], capd[:, :], reads=[capd], writes=[capv])
    p.op("dve", lambda e: e.memset(sc["lo"][:], 0.0), writes=[sc["lo"]])
    p.op("dve", lambda e: e.memset(sc["hi"][:], 1.0), writes=[sc["hi"]])
    S = lambda n: sc[n][:]
    for _ in range(nbis):
        p.op("dve", lambda e: e.tensor_tensor(out=S("mid"), in0=S("lo"), in1=S("hi"), op=ALU.add),
             reads=[sc["lo"], sc["hi"]], writes=[sc["mid"]])
        p.op("dve", lambda e: e.tensor_scalar(out=S("mid"), in0=S("mid"), scalar1=0.5, scalar2=None, op0=ALU.mult),
             reads=[sc["mid"]], writes=[sc["mid"]])
        p.op("dve", lambda e: e.tensor_scalar(out=cmpb[:], in0=ar[:], scalar1=sc["mid"][:, 0:1], scalar2=None, op0=ALU.is_ge),
             reads=[ar, sc["mid"]], writes=[cmpb])
        p.op("dve", lambda e: e.reduce_sum(out=S("cnt"), in_=cmpb[:], axis=AX.X), reads=[cmpb], writes=[sc["cnt"]])
        p.op("dve", lambda e: e.tensor_tensor(out=S("ge"), in0=S("cnt"), in1=capv[:], op=ALU.is_ge),
             reads=[sc["cnt"], capv], writes=[sc["ge"]])
        p.op("dve", lambda e: e.tensor_tensor(out=S("dl"), in0=S("mid"), in1=S("lo"), op=ALU.subtract),
             reads=[sc["mid"], sc["lo"]], writes=[sc["dl"]])
        p.op("dve", lambda e: e.tensor_tensor(out=S("dl"), in0=S("dl"), in1=S("ge"), op=ALU.mult),
             reads=[sc["dl"], sc["ge"]], writes=[sc["dl"]])
        p.op("dve", lambda e: e.tensor_tensor(out=S("lo"), in0=S("lo"), in1=S("dl"), op=ALU.add),
             reads=[sc["lo"], sc["dl"]], writes=[sc["lo"]])
        p.op("dve", lambda e: e.tensor_tensor(out=S("dl"), in0=S("hi"), in1=S("mid"), op=ALU.subtract),
             reads=[sc["hi"], sc["mid"]], writes=[sc["dl"]])
        p.op("dve", lambda e: e.tensor_tensor(out=S("dl"), in0=S("dl"), in1=S("ge"), op=ALU.mult),
             reads=[sc["dl"], sc["ge"]], writes=[sc["dl"]])
        p.op("dve", lambda e: e.tensor_tensor(out=S("hi"), in0=S("mid"), in1=S("dl"), op=ALU.add),
             reads=[sc["mid"], sc["dl"]], writes=[sc["hi"]])
    p.dma("sp", thrd[0:1, :].rearrange("o p -> p o"), sc["lo"][:], reads=[sc["lo"]], writes=[thrd])
    thrB = p.sb([128, P2], F32, "thrB")
    p.dma("sp", thrB[:], thrd[0:1, :].partition_broadcast(128), reads=[thrd], writes=[thrB])

    ntile = NT // 128
    at = p.sb([128, ntile, 2], F32, "at")
    gm = p.sb([128, ntile, 2], F32, "gm")
    p.dma("sp", at[:], atd[:, :].rearrange("(t p) e -> p t e", p=128), reads=[atd], writes=[at])
    for si, (t0, n, cap) in enumerate(sets):
        for e_ in range(2):
            col = e_ * ns + si
            ts = slice(t0 // 128, (t0 + n) // 128)
            p.op("dve", lambda e, ts=ts, e_=e_, col=col: e.scalar_tensor_tensor(
                out=gm[:, ts, e_], in0=at[:, ts, e_], scalar=thrB[:, col:col + 1], in1=at[:, ts, e_],
                op0=ALU.is_ge, op1=ALU.mult), reads=[at, thrB], writes=[gm])

    pp = PsPool(p, 6)
    hx = [p.sb([128, 16, 512], BF16, "hx%d" % i) for i in range(2)]
    w1b = p.sb([128, 16, 512], BF16, "w1b")
    w3b = p.sb([128, 16, 512], BF16, "w3b")
    w2b = [p.sb([128, 16, 512], BF16, "w2b%d" % i) for i in range(2)]
    hT = p.sb([128, 32, 512], BF16, "hT")
    sa = p.sb([128, 512], F32, "sa")
    osb = p.sb([128, 4, 512], F32, "osb")
    otmp = p.sb([128, 512], F32, "otmp")
    for bi, b0 in enumerate(range(0, NT, 512)):
        bn = min(512, NT - b0)
        hb = hx[bi % 2]
        p.dma("sp", hb[:, :, 0:bn], hxd[:, b0:b0 + bn].rearrange("(k p) t -> p k t", p=128), reads=[hxd], writes=[hb])
        for e_ in range(2):
            for fb in range(4):
                fs = slice(fb * 512, (fb + 1) * 512)
                p.dma("pool", w1b[:], wd[0][e_, :, fs].rearrange("(k p) c -> p k c", p=128), reads=[wd[0]], writes=[w1b])
                p.dma("pool", w3b[:], wd[1][e_, :, fs].rearrange("(k p) c -> p k c", p=128), reads=[wd[1]], writes=[w3b])
                for fi in range(4):
                    pa, pg = pp.next(), pp.next()
                    for k in range(16):
                        p.mm(pa, pa[:, 0:bn], w1b, w1b[:, k, fi * 128:(fi + 1) * 128], hb, hb[:, k, 0:bn],
                             start=(k == 0), stop=(k == 15))
                    for k in range(16):
                        p.mm(pg, pg[:, 0:bn], w3b, w3b[:, k, fi * 128:(fi + 1) * 128], hb, hb[:, k, 0:bn],
                             start=(k == 0), stop=(k == 15))
                    p.op("act", lambda e, pa=pa, bn=bn: e.activation(out=sa[:, 0:bn], in_=pa[:, 0:bn], func=AF.Silu),
                         reads=[pa], writes=[sa])
                    fidx = e_ * 16 + fb * 4 + fi
                    p.op("dve", lambda e, pg=pg, bn=bn, fidx=fidx: e.tensor_tensor(
                        out=hT[:, fidx, 0:bn], in0=sa[:, 0:bn], in1=pg[:, 0:bn], op=ALU.mult), reads=[sa, pg], writes=[hT])
        for dq in range(4):
            ds_ = slice(dq * 512, (dq + 1) * 512)
            for e_ in range(2):
                p.dma("pool", w2b[e_][:], wd[2][e_, :, ds_].rearrange("(k p) c -> p k c", p=128), reads=[wd[2]],
                      writes=[w2b[e_]])
            for ti in range(bn // 128):
                tg = b0 // 128 + ti
                pso = [pp.next(), pp.next()]
                for e_ in range(2):
                    for f in range(16):
                        p.mm(pso[e_], pso[e_][:], hT, hT[:, e_ * 16 + f, ti * 128:(ti + 1) * 128], w2b[e_], w2b[e_][:, f, :],
                             start=(f == 0), stop=(f == 15))
                p.op("act", lambda e, pso=pso, tg=tg: e.activation(out=otmp[:], in_=pso[0][:], func=AF.Copy,
                                                                   scale=gm[:, tg, 0:1]), reads=[pso[0], gm], writes=[otmp])
                p.op("dve", lambda e, pso=pso, tg=tg, ti=ti: e.scalar_tensor_tensor(
                    out=osb[:, ti, :], in0=pso[1][:], scalar=gm[:, tg, 1:2], in1=otmp[:], op0=ALU.mult, op1=ALU.add),
                    reads=[pso[1], gm, otmp], writes=[osb])
            p.dma("sp", part[b0:b0 + bn, ds_].rearrange("(t p) c -> p t c", p=128), osb[:, 0:bn // 128, :],
                  reads=[osb], writes=[part])
    return p


def build_final(kinds, last):
    p = Prog()
    nt = len(kinds)
    NT = nt * 128
    parts = p.dram("parts", [8, NT, D], F32, "ExternalInput")
    xnew = p.dram("xnew", [NT, D], F32, "ExternalInput")
    modd = {"x": p.dram("modx", [6, D], F32, "ExternalInput"), "c": p.dram("modc", [6, D], F32, "ExternalInput")}
    fg = p.dram("fg", [1, D], F32, "ExternalInput")
    out = p.dram("xout", [NT, D], F32, "ExternalOutput")
    m5 = p.sb([128, D], F32, "m5")
    fgb = p.sb([128, D], F32, "fgb")
    zs = p.sb([128, D], F32, "zs")
    p.dma("sp", fgb[:], fg[0:1, :].partition_broadcast(128), reads=[fg], writes=[fgb])
    p.op("pool", lambda e: e.memset(zs[:], 0.0), writes=[zs])
    ln = LN(p)
    xt = [p.sb([128, D], F32, "xt%d" % i) for i in range(2)]
    pt = [p.sb([128, D], F32, "pt%d" % i) for i in range(3)]
    acc = p.sb([128, D], F32, "acc")
    ob = p.sb([128, D], F32, "ob")
    cur = None
    for tg in range(nt):
        if kinds[tg] != cur:
            cur = kinds[tg]
            p.dma("sp", m5[:], modd[cur][5:6, :].partition_broadcast(128), reads=[modd[cur]], writes=[m5])
        rows = slice(tg * 128, (tg + 1) * 128)
        x_ = xt[tg % 2]
        p.dma("sp", x_[:], xnew[rows, :], reads=[xnew], writes=[x_])
        for k in range(8):
            b_ = pt[k % 3]
            p.dma("sp", b_[:], parts[k, rows, :], reads=[parts], writes=[b_])
            eng = "dve" if k % 2 == 0 else "pool"
            if k == 0:
                p.op("dve", lambda e, b_=b_: e.tensor_copy(out=acc[:], in_=b_[:]), reads=[b_], writes=[acc])
            else:
                p.op(eng, lambda e, b_=b_: e.tensor_tensor(out=acc[:], in0=acc[:], in1=b_[:], op=ALU.add),
                     reads=[acc, b_], writes=[acc])
        p.op("dve", lambda e: e.tensor_tensor(out=acc[:], in0=acc[:], in1=m5[:], op=ALU.mult), reads=[acc, m5], writes=[acc])
        p.op("pool", lambda e, x_=x_: e.tensor_tensor(out=x_[:], in0=x_[:], in1=acc[:], op=ALU.add), reads=[x_, acc], writes=[x_])
        if last:
            ln.run(x_, x_[:], fgb, zs, ob, ob[:])
            p.dma("sp", out[rows, :], ob[:], reads=[ob], writes=[out])
        else:
            p.dma("sp", out[rows, :], x_[:], reads=[x_], writes=[out])
    return p


N_MIX_IN = 4352
import os as _os


def _chk(tag, res, names):
    if not _os.environ.get("KDEBUG"):
        return
    bad = False
    for n in names:
        fin = [bool(np.isfinite(np.asarray(r[n], dtype=np.float32)).all()) for r in res]
        mx = max(float(np.nanmax(np.abs(np.asarray(r[n], dtype=np.float32)))) for r in res)
        print("KDEBUG", tag, n, "finite per core", fin, "absmax", mx, flush=True)
        bad = bad or not all(fin)
    if bad:
        raise RuntimeError("KDEBUG: first non-finite stage = " + tag)


def kernel(x, c, ctx, c_ctx, ada_w, ada_b, norm1_g, norm2_g, w_in, da_lambda, da_subln_g, wb_sink, na_rpb,
           s5_a_re, s5_a_im, s5_log_step, s5_b_re, s5_b_im, s5_c_re, s5_c_im, s5_d, s5_glu_w,
           w_branch, w_out, w_router, w_e1, w_e3, w_e2, final_g):
    A = lambda a: np.ascontiguousarray(np.asarray(a, dtype=np.float32))
    x, c, ctx, c_ctx = A(x), A(c), A(ctx), A(c_ctx)
    xs = [x[b].copy() for b in range(4)]
    cs = [ctx[b].copy() for b in range(4)]
    mod = run_ada(c, c_ctx, np.asarray(ada_w, np.float32), np.asarray(ada_b, np.float32))
    cosT, sinT = rope_tables()
    ident = np.eye(128, dtype=np.float32)
    nm, na_idx = na_tables()
    nam = np.ascontiguousarray(np.tile(nm, (1, 1, 4)))
    wbm = wb_masks()
    pcols = [proj_cols(0), proj_cols(1)]
    for l in range(2):
        last = l == 1
        modl = mod[l].reshape(5, 6, D)
        g1, g2 = A(norm1_g[l])[None], A(norm2_g[l])[None]
        wl = np.asarray(w_in[l], np.float32)
        wsel = [np.ascontiguousarray(wl[:, pcols[h]]) for h in range(2)]
        maps = []
        for core in range(8):
            b, h = core // 2, core % 2
            maps.append({"xin": np.concatenate([cs[b], xs[b]], 0), "modx": A(modl[b]), "modc": A(modl[4]), "g1": g1,
                         "wsel": wsel[h], "cos": cosT, "sin": sinT, "ident": ident})
        r1 = run(build_proj(), maps)
        _chk("L%d B1" % l, r1, ("qkT", "v"))
        lam_init = 0.8 - 0.6 * math.exp(-0.3 * l)
        maps = []
        for core in range(8):
            h = core % 2
            maps.append({"qkT": r1[core]["qkT"], "v": r1[core]["v"], "lamb": A(da_lambda[l]).reshape(1, 256),
                         "gsub": A(da_subln_g[l])[None], "sink": A(wb_sink[l])[h * 4:(h + 1) * 4][None], "wbm": wbm,
                         "nam": nam, "rpbT": na_rpb_gather(A(na_rpb[l])[h * 4:(h + 1) * 4])})
        r2 = run(build_attn(lam_init, na_idx, nm.shape[0]), maps)
        _chk("L%d B2" % l, r2, ("yda", "ywb", "yna"))
        s5h = [s5_host(h, A(s5_a_re[l]), A(s5_a_im[l]), A(s5_log_step[l]), A(s5_b_re[l]), A(s5_b_im[l]),
                       A(s5_c_re[l]), A(s5_c_im[l]), A(s5_d[l])) for h in range(2)]
        maps = []
        for core in range(8):
            m_ = dict(s5h[core % 2])
            m_["uT"] = np.ascontiguousarray(r1[core]["qkT"][11:13])
            maps.append(m_)
        r3 = run(build_s5(), maps)
        _chk("L%d B3" % l, r3, ("gT",))
        ys = []
        for b in range(4):
            cols = []
            for nme in ("yda", "ywb", "yna"):
                cols += [r2[2 * b][nme], r2[2 * b + 1][nme]]
            cols += [r3[2 * b]["gT"].reshape(256, NTOK).T, r3[2 * b + 1]["gT"].reshape(256, NTOK).T]
            ys.append(np.concatenate(cols, 1))
        kinds = ["x"] * 8 + ([] if last else ["c"])
        wgt = np.ascontiguousarray(wl[:, N_MIX_IN:].reshape(D, 4, 16, 128).transpose(2, 0, 1, 3).reshape(16, D, 512))
        wbr = np.ascontiguousarray(A(w_branch[l]).reshape(4, 512, 16, 128).transpose(2, 0, 1, 3).reshape(16, 2048, 128))
        maps = []
        for core in range(8):
            b, hf = core // 2, core % 2
            xr = slice(hf * 1024, (hf + 1) * 1024)
            cr = slice(hf * 128, (hf + 1) * 128)
            xin = xs[b][xr] if last else np.concatenate([xs[b][xr], cs[b][cr]], 0)
            yr = ys[b][256 + hf * 1024:256 + (hf + 1) * 1024]
            if not last:
                yr = np.concatenate([yr, ys[b][cr]], 0)
            maps.append({"xin": np.ascontiguousarray(xin), "yT": np.ascontiguousarray(yr.T), "modx": A(modl[b]),
                         "modc": A(modl[4]), "g1": g1, "g2": g2, "wg": wgt, "wbr": wbr, "wout": A(w_out[l]),
                         "wglu": A(s5_glu_w[l]), "wr": A(w_router[l]), "ident": ident})
        rC = run(build_merge(kinds, 3), maps)
        _chk("L%d C" % l, rC, ("xnew", "hx2", "aff"))
        hx2 = [np.concatenate([rC[2 * b]["hx2"][:1024], rC[2 * b + 1]["hx2"][:1024]], 0) for b in range(4)]
        aff = [np.concatenate([rC[2 * b]["aff"][:1024], rC[2 * b + 1]["aff"][:1024]], 0) for b in range(4)]
        sets = [(b * 2048, 2048, 256) for b in range(4)]
        if not last:
            hx2 += [np.concatenate([rC[2 * b]["hx2"][1024:], rC[2 * b + 1]["hx2"][1024:]], 0) for b in range(4)]
            aff += [np.concatenate([rC[2 * b]["aff"][1024:], rC[2 * b + 1]["aff"][1024:]], 0) for b in range(4)]
            sets += [(8192 + b * 256, 256, 32) for b in range(4)]
        hx2 = np.concatenate(hx2, 0)
        aff = np.concatenate(aff, 0)
        NT = hx2.shape[0]
        hxT = np.ascontiguousarray(hx2.T)
        ns = len(sets)
        maps = []
        for core in range(8):
            affR = np.full((2 * ns, 2048), -1.0, np.float32)
            capv = np.zeros((2 * ns, 1), np.float32)
            for e_ in range(2):
                for si, (t0, n, cap) in enumerate(sets):
                    affR[e_ * ns + si, :n] = aff[t0:t0 + n, 2 * core + e_]
                    capv[e_ * ns + si, 0] = cap
            maps.append({"hxT": hxT, "affR": affR, "capv": capv, "affT": np.ascontiguousarray(aff[:, 2 * core:2 * core + 2]),
                         "w1": A(w_e1[l][2 * core:2 * core + 2]), "w3": A(w_e3[l][2 * core:2 * core + 2]),
                         "w2": A(w_e2[l][2 * core:2 * core + 2])})
        rD = run(build_moe(sets, NT), maps)
        _chk("L%d D" % l, rD, ("part",))
        maps = []
        for core in range(8):
            b, hf = core // 2, core % 2
            idx = b * 2048 + hf * 1024 + np.arange(1024)
            if not last:
                idx = np.concatenate([idx, 8192 + b * 256 + hf * 128 + np.arange(128)])
            maps.append({"parts": np.ascontiguousarray(np.stack([rD[k]["part"][idx] for k in range(8)])),
                         "xnew": rC[core]["xnew"], "modx": A(modl[b]), "modc": A(modl[4]), "fg": A(final_g)[None]})
        rE = run(build_final(kinds, last), maps)
        _chk("L%d E" % l, rE, ("xout",))
        for core in range(8):
            b, hf = core // 2, core % 2
            xs[b][hf * 1024:(hf + 1) * 1024] = rE[core]["xout"][:1024]
            if not last:
                cs[b][hf * 128:(hf + 1) * 128] = rE[core]["xout"][1024:]
    return np.stack(xs).astype(np.float32)
```
